# Optimizing a Trainium2 kernel written in Bass

```python
import math
import jax, jax.numpy as jnp
from jax import lax
import numpy as np

D_MODEL = 1024
BATCH = 4
SEQ = 8192
DEPTH = 1

HEAD_DIM = 64
N_HEADS_A = 8
N_HEADS_B = 8
WIDTH_A = N_HEADS_A * HEAD_DIM
WIDTH_B = N_HEADS_B * HEAD_DIM
ROPE_DIM = HEAD_DIM // 4
ROPE_THETA = 500000.0
IDX_HEADS = 8
IDX_DIM = 64
TOPK_MAX = 256
BLOCK_Q = 128
D_FF = 4 * D_MODEL
EPS = 1e-6
IN_SPLITS = (WIDTH_A, WIDTH_A, WIDTH_A,
             WIDTH_B, WIDTH_B, WIDTH_B,
             IDX_HEADS * IDX_DIM, IDX_DIM,
             IDX_HEADS,
             D_MODEL, D_MODEL)
IN_COLS = sum(IN_SPLITS)

kernel_name = "hybrid_dsa_stickbreaking_sqrelu_block"


def rmsnorm(x, g):
    x32 = x.astype(jnp.float32)
    y = x32 * lax.rsqrt(jnp.mean(x32 * x32, axis=-1, keepdims=True) + EPS)
    return (y * g.astype(jnp.float32)).astype(x.dtype)


def rope_tables(seq_len):
    pos = jnp.arange(seq_len, dtype=jnp.float32)
    inv_freq = ROPE_THETA ** (-jnp.arange(0, ROPE_DIM, 2, dtype=jnp.float32) / ROPE_DIM)
    ang = pos[:, None] * inv_freq[None, :]
    return jnp.cos(ang), jnp.sin(ang)


def partial_rope(x, cos, sin):
    half = ROPE_DIM // 2
    x1 = x[..., :half]
    x2 = x[..., half:ROPE_DIM]
    cos = cos.astype(x.dtype)
    sin = sin.astype(x.dtype)
    return jnp.concatenate([x1 * cos - x2 * sin, x2 * cos + x1 * sin, x[..., ROPE_DIM:]], axis=-1)


def to_blocks(a, nblk):
    return a.reshape((a.shape[0], nblk, BLOCK_Q) + a.shape[2:]).swapaxes(0, 1)


def from_blocks(a):
    a = a.swapaxes(0, 1)
    return a.reshape((a.shape[0], a.shape[1] * a.shape[2]) + a.shape[3:])


def token_mixers(q_a, k_a, v_a, q_b, k_b, v_b, q_i, k_i, w_i):
    L = q_a.shape[1]
    nblk = L // BLOCK_Q
    topk = min(TOPK_MAX, L // 4)
    f32 = jnp.float32
    key_pos = jnp.arange(L, dtype=jnp.int32)
    scale = HEAD_DIM ** -0.5
    idx_scale = (IDX_DIM ** -0.5) * (IDX_HEADS ** -0.5)

    def block_fn(args):
        qa, qi, wi, qb, t0 = args
        t = t0 + jnp.arange(BLOCK_Q, dtype=jnp.int32)
        rel = jax.nn.relu(jnp.einsum('bqhd,bsd->bqhs', qi, k_i).astype(f32))
        isc = jnp.einsum('bqhs,bqh->bqs', rel, wi.astype(f32)) * idx_scale
        causal = key_pos[None, :] <= t[:, None]
        isc = jnp.where(causal[None], isc, -jnp.inf)
        _, sel = lax.top_k(isc, topk)
        valid = sel <= t[None, :, None]
        kg = jax.vmap(lambda kb, ib: kb[ib])(k_a, sel)
        vg = jax.vmap(lambda vb, ib: vb[ib])(v_a, sel)
        logit = jnp.einsum('bqhd,bqkhd->bqhk', qa, kg).astype(f32) * scale
        logit = jnp.where(valid[:, :, None, :], logit, -jnp.inf)
        p = jax.nn.softmax(logit, axis=-1).astype(vg.dtype)
        oa = jnp.einsum('bqhk,bqkhd->bqhd', p, vg)
        z = jnp.einsum('bqhd,bshd->bhqs', qb, k_b).astype(f32) * scale
        strict = (key_pos[None, :] < t[:, None])[None, None]
        log_1mb = jnp.where(strict, jax.nn.log_sigmoid(-z), 0.0)
        after = lax.cumsum(log_1mb, axis=3, reverse=True) - log_1mb
        att = jnp.where(strict, jnp.exp(jax.nn.log_sigmoid(z) + after), 0.0)
        ob = jnp.einsum('bhqs,bshd->bqhd', att.astype(v_b.dtype), v_b)
        return oa, ob

    starts = jnp.arange(nblk, dtype=jnp.int32) * BLOCK_Q
    oa, ob = lax.map(block_fn, (to_blocks(q_a, nblk), to_blocks(q_i, nblk),
                                to_blocks(w_i, nblk), to_blocks(q_b, nblk), starts))
    return from_blocks(oa), from_blocks(ob)


def setup_inputs(seed: int = 0) -> dict:
    key = jax.random.key(seed)
    ks = jax.random.split(key, 16)
    f32 = jnp.float32
    nrm = lambda k, shape, fan_in: jax.random.normal(k, shape, f32) * (fan_in ** -0.5)
    gain = lambda k: 1.0 + 0.05 * jax.random.normal(k, (DEPTH, D_MODEL), f32)
    return {
        "x": jax.random.normal(ks[0], (BATCH, SEQ, D_MODEL), f32),
        "c": jax.random.normal(ks[1], (BATCH, D_MODEL), f32),
        "w_ada": nrm(ks[2], (DEPTH, D_MODEL, 6 * D_MODEL), D_MODEL),
        "b_ada": 0.01 * jax.random.normal(ks[3], (DEPTH, 6 * D_MODEL), f32),
        "g_pre_mix": gain(ks[4]),
        "w_in": nrm(ks[5], (DEPTH, D_MODEL, IN_COLS), D_MODEL),
        "w_up_a": nrm(ks[6], (DEPTH, WIDTH_A, D_MODEL), WIDTH_A),
        "w_up_b": nrm(ks[7], (DEPTH, WIDTH_B, D_MODEL), WIDTH_B),
        "w_out": nrm(ks[8], (DEPTH, D_MODEL, D_MODEL), D_MODEL),
        "g_post_mix": gain(ks[9]),
        "g_pre_ffn": gain(ks[10]),
        "w_ff1": nrm(ks[11], (DEPTH, D_MODEL, D_FF), D_MODEL),
        "w_ff2": nrm(ks[12], (DEPTH, D_FF, D_MODEL), D_FF),
        "g_post_ffn": gain(ks[13]),
    }


def reference(x, c, w_ada, b_ada, g_pre_mix, w_in, w_up_a, w_up_b, w_out, g_post_mix,
              g_pre_ffn, w_ff1, w_ff2, g_post_ffn):
    B, L, D = x.shape
    cos, sin = rope_tables(L)
    cos_h, sin_h = cos[:, None, :], sin[:, None, :]
    split_at = list(np.cumsum(IN_SPLITS)[:-1])
    for l in range(DEPTH):
        mod = jax.nn.silu(c) @ w_ada[l] + b_ada[l]
        sh1, sc1, ga1, sh2, sc2, ga2 = [m[:, None, :] for m in jnp.split(mod, 6, axis=-1)]

        h = rmsnorm(x, g_pre_mix[l]) * (1.0 + sc1) + sh1
        proj = h @ w_in[l]
        qa, ka, va, qb, kb, vb, qi, ki, wi, gate_a, gate_b = jnp.split(proj, split_at, axis=-1)
        heads = lambda t, n: t.reshape(B, L, n, HEAD_DIM)
        qa = partial_rope(heads(qa, N_HEADS_A), cos_h, sin_h)
        ka = partial_rope(heads(ka, N_HEADS_A), cos_h, sin_h)
        va = heads(va, N_HEADS_A)
        qb, kb, vb = heads(qb, N_HEADS_B), heads(kb, N_HEADS_B), heads(vb, N_HEADS_B)
        qi = partial_rope(qi.reshape(B, L, IDX_HEADS, IDX_DIM), cos_h, sin_h)
        ki = partial_rope(ki, cos, sin)
        oa, ob = token_mixers(qa, ka, va, qb, kb, vb, qi, ki, wi)
        ya = oa.reshape(B, L, WIDTH_A) @ w_up_a[l]
        yb = ob.reshape(B, L, WIDTH_B) @ w_up_b[l]
        merged = jax.nn.sigmoid(gate_a) * ya + jax.nn.sigmoid(gate_b) * yb
        y = merged @ w_out[l]
        x = x + ga1 * rmsnorm(y, g_post_mix[l])

        h = rmsnorm(x, g_pre_ffn[l]) * (1.0 + sc2) + sh2
        u = jax.nn.relu(h @ w_ff1[l])
        y = (u * u) @ w_ff2[l]
        x = x + ga2 * rmsnorm(y, g_post_ffn[l])
    return x
```

```python
import os
import numpy as np
import concourse.bass as bass
import concourse.mybir as mybir
from concourse.bass_utils import run_bass_kernel_spmd

F32 = mybir.dt.float32
BF16 = mybir.dt.bfloat16
AF = mybir.ActivationFunctionType
ALU = mybir.AluOpType
AX = mybir.AxisListType

D = 1024
L = 8192
NB = 64
OWN = 4096
NQB = 32
H = 8
DH = 64
DFF = 4096
EPS = 1e-6
NEG = -30000.0
NBIS = 16
ARENA_F32 = 52000

NK = 2816
NQ = 4616


class Buf:
    __slots__ = ("ap", "lw", "rd", "wsem", "wcnt", "rsem", "rcnt", "name")

    def __init__(self, ap, name=""):
        self.ap = ap
        self.lw = None
        self.rd = []
        self.wsem = None
        self.wcnt = 0
        self.rsem = None
        self.rcnt = 0
        self.name = name


class K:
    ENG = ("pe", "act", "dve", "pool", "sp")

    def __init__(self, nc):
        self.nc = nc
        self.ops = {e: [] for e in self.ENG}
        self.cnt = {e: 0 for e in self.ENG}
        self.waited = {e: {} for e in self.ENG}
        self.dma_keys = []
        self.dma_cnt = {}
        self.nsem = 0
        self.store_key = self.new_dma_key()

    def new_dma_key(self):
        k = "dma%d" % len(self.dma_keys)
        self.dma_keys.append(k)
        self.dma_cnt[k] = 0
        return k

    def _waits(self, eng, toks, same_engine_raw):
        ws = []
        wd = self.waited[eng]
        for t in toks:
            if t is None:
                continue
            key, val = t
            if key == eng and not same_engine_raw:
                continue
            if wd.get(key, 0) >= val:
                continue
            wd[key] = val
            ws.append((key, val))
        return ws

    def op(self, eng, fn, reads=(), writes=(), extra=()):
        toks = list(extra)
        raw = []
        for b in reads:
            if b.lw is not None:
                raw.append(b.lw)
        war = []
        for b in writes:
            if b.lw is not None:
                war.append(b.lw)
            war.extend(b.rd)
        ws = self._waits(eng, [t for t in raw if not (t[0] == eng and eng == "pe")], True)
        ws += self._waits(eng, [t for t in (war + toks) if t[0] != eng], True)
        self.cnt[eng] += 1
        tok = (eng, self.cnt[eng])
        self.ops[eng].append((fn, ws, 1))
        for b in reads:
            b.rd.append(tok)
        for b in writes:
            b.lw = tok
            b.rd = []
        return tok

    def dma(self, out, in_, reads=(), writes=(), key=None):
        eng = "sp"
        toks = []
        for b in reads:
            if b.lw is not None:
                toks.append(b.lw)
        for b in writes:
            if b.lw is not None:
                toks.append(b.lw)
            toks.extend(b.rd)
        ws = self._waits(eng, toks, True)
        if writes:
            b = writes[0]
            if b.wsem is None:
                b.wsem = self.new_dma_key()
            k = b.wsem
        elif reads:
            b = reads[0]
            if b.rsem is None:
                b.rsem = self.new_dma_key()
            k = b.rsem
        else:
            k = self.store_key
        if key is not None:
            k = key
        self.dma_cnt[k] += 16
        tok = (k, self.dma_cnt[k])
        self.ops[eng].append((lambda e, o=out, i=in_: e.dma_start(out=o, in_=i), ws, (k, 16)))
        for b in reads:
            b.rd.append(tok)
        for b in writes:
            b.lw = tok
            b.rd = []
        return tok

    def barrier(self):
        toks = [(e, self.cnt[e]) for e in self.ENG if self.cnt[e] > 0]
        toks += [(k, v) for k, v in self.dma_cnt.items() if v > 0]
        for e in self.ENG:
            ws = self._waits(e, toks, False)
            if ws:
                self.ops[e].append((None, ws, 0))

    def replay(self):
        nc = self.nc
        keys = list(self.ENG) + self.dma_keys
        import contextlib
        with contextlib.ExitStack() as st:
            sems = {k: st.enter_context(nc.semaphore("s_" + k)) for k in keys}
            block = st.enter_context(nc.Block())

            def run(e, name):
                for fn, ws, sig in self.ops[name]:
                    for key, val in ws:
                        e.wait_ge(sems[key], val)
                    if fn is None:
                        continue
                    ins = fn(e)
                    if sig == 1:
                        ins.then_inc(sems[name], 1)
                    elif sig:
                        ins.then_inc(sems[sig[0]], sig[1])

            @block.tensor
            def _(e):
                run(e, "pe")

            @block.scalar
            def _(e):
                run(e, "act")

            @block.vector
            def _(e):
                run(e, "dve")

            @block.gpsimd
            def _(e):
                run(e, "pool")

            @block.sync
            def _(e):
                run(e, "sp")


class Arena:
    def __init__(self, t, nwords):
        self.t = t
        self.n = nwords
        self.off = 0

    def mark(self):
        return self.off

    def reset(self, m):
        self.off = m

    def alloc(self, n, dt=F32, name=""):
        w = n if dt == F32 else (n + 1) // 2
        assert self.off + w <= self.n, ("arena overflow", name, self.off, w, self.n)
        v = self.t[:, self.off:self.off + w]
        self.off += w
        if dt != F32:
            v = v.bitcast(dt)
        return v


def build_program(debug=False):
    NQB_RUN = int(os.environ.get('MK_NQB', NQB))
    KT_RUN = int(os.environ.get('MK_KT', L // 512))
    QT_RUN = int(os.environ.get('MK_QT', OWN // 512))
    nc = bass.Bass("TRN2", target_bir_lowering=False)

    def din(name, shape, dt=F32):
        return nc.dram_tensor(name, list(shape), dt, kind="ExternalInput").ap()

    skind = "ExternalOutput" if debug else "Internal"

    def dscr(name, shape, dt):
        return nc.dram_tensor(name, list(shape), dt, kind=skind).ap()

    xall = din("xall", [L, D])
    xown = din("xown", [OWN, D])
    ccol = din("ccol", [128, 8])
    w_ada = din("w_ada", [D, 6 * D])
    b_ada = din("b_ada", [1, 6 * D])
    g_rows = din("g_rows", [4, D])
    WK = din("WK", [D, NK])
    WQ = din("WQ", [D, NQ])
    w_up = din("w_up", [D, D])
    w_out = din("w_out", [D, D])
    w_ff1 = din("w_ff1", [D, DFF])
    w_ff2 = din("w_ff2", [DFF, D])
    cosK = din("cosK", [64, L])
    sinK = din("sinK", [64, L])
    cosQ = din("cosQ", [64, OWN])
    sinQ = din("sinQ", [64, OWN])
    cosI = din("cosI", [64, OWN])
    sinI = din("sinI", [64, OWN])
    consts = din("consts", [128, 1536])
    out_own = nc.dram_tensor("out_own", [OWN, D], F32, kind="ExternalOutput").ap()

    KA_T = dscr("KA_T", [H, 64, L], BF16)
    KB_T = dscr("KB_T", [H, 64, L], BF16)
    KI_T = dscr("KI_T", [64, L], BF16)
    VA = dscr("VA", [L, 512], BF16)
    VB = dscr("VB", [L + 128, 512], BF16)
    DVB = dscr("DVB", [L, 512], BF16)
    QA_T = dscr("QA_T", [H, 64, OWN], BF16)
    QB_T = dscr("QB_T", [H, 64, OWN], BF16)
    QI_T = dscr("QI_T", [H, 64, OWN], BF16)
    GA_T = dscr("GA_T", [8, 128, OWN], BF16)
    GB_T = dscr("GB_T", [8, 128, OWN], BF16)
    WI = dscr("WI", [OWN, 8], F32)
    OAB_T = dscr("OAB_T", [8, 128, OWN], BF16)
    X1 = dscr("X1", [OWN, D], F32)

    arena_t = nc.alloc_sbuf_tensor("arena", [128, ARENA_F32], F32)
    A = Arena(arena_t, ARENA_F32)
    banks = [nc.alloc_psum_tensor("ps%d" % i, [128, 512], F32) for i in range(8)]
    PS = [Buf(banks[i][:, :], "ps%d" % i) for i in range(8)]
    k = K(nc)

    def B(n, dt=F32, name=""):
        return Buf(A.alloc(n, dt, name), name)

    cst = B(1536, F32, "cst")
    k.dma(cst.ap, consts[:, :], writes=[cst])
    cstb = B(1536, BF16, "cstb")
    k.op("dve", lambda e: e.tensor_copy(out=cstb.ap, in_=cst.ap), reads=[cst], writes=[cstb])
    ident_f = cst.ap[:, 0:128]
    ident_b = cstb.ap[:, 0:128]
    onehot_b = cstb.ap[:, 128:256]
    maskI = [cst.ap[:, 256:512], cst.ap[:, 512:768]]
    maskS = [cstb.ap[:, 768:1024], cstb.ap[:, 1024:1280]]
    ones_f = cst.ap[:, 1280:1408]
    cols = B(33, F32, "cols")
    A1bc = B(1024, F32, "A1bc")
    A2bc = B(1024, F32, "A2bc")

    m0 = A.mark()
    cc = B(8, F32, "cc")
    k.dma(cc.ap, ccol[:, :], writes=[cc])
    sg = B(8, F32, "sg")
    k.op("act", lambda e: e.activation(out=sg.ap, in_=cc.ap, func=AF.Silu), reads=[cc], writes=[sg])
    modr = B(6 * D, F32, "modr")
    bar = B(6 * D, F32, "bar")
    k.dma(bar.ap, b_ada[0:1, :].broadcast_to([128, 6 * D]), writes=[bar])
    gr = B(4 * D, F32, "gr")
    for i in range(4):
        k.dma(gr.ap[:, i * D:(i + 1) * D], g_rows[i:i + 1, :].broadcast_to([128, D]), writes=[gr])
    sgb = B(8 * 128, F32, "sgb")
    sgv = sgb.ap.rearrange("p (k n) -> p k n", k=8)
    for kk in range(8):
        k.op("dve", lambda e, kk=kk: e.tensor_scalar(out=sgv[:, kk, :], in0=ones_f, scalar1=sg.ap[:, kk:kk + 1], scalar2=None, op0=ALU.mult),
             reads=[sg, cst], writes=[sgb])
    wst = [B(512, F32, "wst%d" % i) for i in range(4)]
    ni = 0
    for cchunk in range(12):
        ps = PS[cchunk % 2]
        for kk in range(8):
            w = wst[ni % 4]
            ni += 1
            k.dma(w.ap, w_ada[kk * 128:(kk + 1) * 128, cchunk * 512:(cchunk + 1) * 512], writes=[w])
            k.op("pe", lambda e, ps=ps, w=w, kk=kk: e.matmul(ps.ap, lhsT=sgv[:, kk, :], rhs=w.ap, start=(kk == 0), stop=(kk == 7)),
                 reads=[sgb, w], writes=[ps])
        k.op("dve", lambda e, ps=ps, c=cchunk: e.tensor_tensor(out=modr.ap[:, c * 512:(c + 1) * 512], in0=ps.ap,
                                                              in1=bar.ap[:, c * 512:(c + 1) * 512], op=ALU.add),
             reads=[ps, bar], writes=[modr])
    rows = B(2 * D, F32, "rows")

    def mrow(i):
        return modr.ap[:, i * D:(i + 1) * D]

    k.op("dve", lambda e: e.scalar_tensor_tensor(out=rows.ap[:, 0:D], in0=mrow(1), scalar=1.0, in1=gr.ap[:, 0:D],
                                                 op0=ALU.add, op1=ALU.mult), reads=[modr, gr], writes=[rows])
    k.op("dve", lambda e: e.scalar_tensor_tensor(out=rows.ap[:, D:2 * D], in0=mrow(4), scalar=1.0, in1=gr.ap[:, 2 * D:3 * D],
                                                 op0=ALU.add, op1=ALU.mult), reads=[modr, gr], writes=[rows])
    k.op("dve", lambda e: e.tensor_tensor(out=A1bc.ap, in0=mrow(2), in1=gr.ap[:, D:2 * D], op=ALU.mult),
         reads=[modr, gr], writes=[A1bc])
    k.op("dve", lambda e: e.tensor_tensor(out=A2bc.ap, in0=mrow(5), in1=gr.ap[:, 3 * D:4 * D], op=ALU.mult),
         reads=[modr, gr], writes=[A2bc])
    srcs = [rows.ap[:, 0:D], mrow(0), rows.ap[:, D:2 * D], mrow(3)]
    dtmp = B(128, F32, "dtmp")
    for si in range(4):
        for kk in range(8):
            k.op("dve", lambda e, si=si, kk=kk: e.tensor_tensor(out=dtmp.ap, in0=srcs[si][:, kk * 128:(kk + 1) * 128], in1=ident_f, op=ALU.mult),
                 reads=[rows, modr, cst], writes=[dtmp])
            k.op("dve", lambda e, si=si, kk=kk: e.tensor_reduce(out=cols.ap[:, si * 8 + kk:si * 8 + kk + 1], in_=dtmp.ap, axis=AX.X, op=ALU.add),
                 reads=[dtmp], writes=[cols])
    k.op("pool", lambda e: e.memset(cols.ap[:, 32:33], EPS), reads=[cols], writes=[cols])
    k.barrier()
    A.reset(m0)

    rr = {"cast": 0, "cp": 0}

    def load_weight_bf16(dst, src, nrow_chunks, ncols, stage):
        dv = dst.ap.rearrange("p (k n) -> p k n", k=nrow_chunks)
        for kk in range(nrow_chunks):
            c0 = 0
            while c0 < ncols:
                cw = min(1024, ncols - c0)
                s = stage[rr["cast"] % len(stage)]
                eng = ("dve", "act", "pool")[rr["cast"] % 3]
                rr["cast"] += 1
                k.dma(s.ap[:, 0:cw], src[kk * 128:(kk + 1) * 128, c0:c0 + cw], writes=[s])
                if eng == "act":
                    k.op("act", lambda e, s=s, kk=kk, c0=c0, cw=cw: e.activation(out=dv[:, kk, c0:c0 + cw], in_=s.ap[:, 0:cw], func=AF.Identity),
                         reads=[s], writes=[dst])
                else:
                    k.op(eng, lambda e, s=s, kk=kk, c0=c0, cw=cw: e.tensor_copy(out=dv[:, kk, c0:c0 + cw], in_=s.ap[:, 0:cw]),
                         reads=[s], writes=[dst])
                c0 += cw

    def norm_transpose(xt, hT, col0, sbi, ntok_tile, junk, stat, hn, gi):
        hv = hT.ap.rearrange("p (k n) -> p k n", k=8)
        k.op("act", lambda e: e.activation(out=junk.ap, in_=xt.ap, func=AF.Square, accum_out=stat.ap[:, 0:1]),
             reads=[xt], writes=[junk, stat])
        k.op("act", lambda e: e.activation(out=stat.ap[:, 1:2], in_=stat.ap[:, 0:1], func=AF.Sqrt, scale=1.0 / D, bias=cols.ap[:, 32:33]),
             reads=[stat, cols], writes=[stat])
        k.op("dve", lambda e: e.reciprocal(out=stat.ap[:, 2:3], in_=stat.ap[:, 1:2]), reads=[stat], writes=[stat])
        k.op("act", lambda e: e.activation(out=hn.ap, in_=xt.ap, func=AF.Identity, scale=stat.ap[:, 2:3]),
             reads=[xt, stat], writes=[hn])
        for half in range(2):
            ps = PS[6 + half]
            for q in range(4):
                kk = half * 4 + q
                k.op("pe", lambda e, ps=ps, q=q, kk=kk: e.matmul(ps.ap[:, q * 128:(q + 1) * 128],
                                                                lhsT=hn.ap[:, kk * 128:(kk + 1) * 128], rhs=ident_b,
                                                                start=True, stop=True),
                     reads=[hn, cstb], writes=[ps])
            for q in range(4):
                kk = half * 4 + q
                k.op("act", lambda e, ps=ps, q=q, kk=kk: e.activation(
                    out=hv[:, kk, col0 + sbi * 128:col0 + (sbi + 1) * 128], in_=ps.ap[:, q * 128:(q + 1) * 128],
                    func=AF.Identity, scale=cols.ap[:, gi * 8 + kk:gi * 8 + kk + 1],
                    bias=cols.ap[:, (gi + 1) * 8 + kk:(gi + 1) * 8 + kk + 1]),
                     reads=[ps, cols], writes=[hT])

    def proj_phase(xsrc, ntiles, Wd, ncols, groups_fn, cos_d, sin_d, cosi_d=None, sini_d=None):
        m = A.mark()
        Wb = B(8 * ncols, BF16, "Wb")
        stage = [B(1024, F32, "wstage%d" % i) for i in range(2)]
        load_weight_bf16(Wb, Wd, 8, ncols, stage)
        Wv = Wb.ap.rearrange("p (k n) -> p k n", k=8)
        xt = [B(D, F32, "xt%d" % i) for i in range(2)]
        junk = B(D, BF16, "junk")
        stat = B(4, F32, "stat")
        hn = B(D, BF16, "hn")
        hT_l = [B(8 * 512, BF16, "hT%d" % i) for i in range(2)]
        ctab_l = [B(512, F32, "ctab%d" % i) for i in range(2)]
        stab_l = [B(512, F32, "stab%d" % i) for i in range(2)]
        ctab2_l = [B(512, F32, "ctab2_%d" % i) for i in range(2)]
        stab2_l = [B(512, F32, "stab2_%d" % i) for i in range(2)]
        t1 = B(512, F32, "t1")
        t2 = B(512, F32, "t2")
        ost = [B(512, BF16, "ost%d" % i) for i in range(4)]
        wist = B(8, F32, "wist")
        oi = [0]

        def norm_part(ti):
            tok0 = ti * 512
            hT = hT_l[ti % 2]
            th = []
            for sbi in range(4):
                def f(sbi=sbi):
                    x_ = xt[sbi % 2]
                    k.dma(x_.ap, xsrc[tok0 + sbi * 128:tok0 + (sbi + 1) * 128, :], writes=[x_])
                    norm_transpose(x_, hT, 0, sbi, 512, junk, stat, hn, 0)
                th.append(f)

            def tabs():
                ctab, stab, ctab2, stab2 = ctab_l[ti % 2], stab_l[ti % 2], ctab2_l[ti % 2], stab2_l[ti % 2]
                for hh in range(2):
                    k.dma(ctab.ap[hh * 64:(hh + 1) * 64, :], cos_d[:, tok0:tok0 + 512], writes=[ctab])
                    k.dma(stab.ap[hh * 64:(hh + 1) * 64, :], sin_d[:, tok0:tok0 + 512], writes=[stab])
                if cosi_d is not None:
                    for hh in range(2):
                        k.dma(ctab2.ap[hh * 64:(hh + 1) * 64, :], cosi_d[:, tok0:tok0 + 512], writes=[ctab2])
                        k.dma(stab2.ap[hh * 64:(hh + 1) * 64, :], sini_d[:, tok0:tok0 + 512], writes=[stab2])
            th.append(tabs)
            return th

        if True:
            def do_group(g, ti):
                tok0 = ti * 512
                hT = hT_l[ti % 2]
                hv = hT.ap.rearrange("p (k n) -> p k n", k=8)
                ctab, stab, ctab2, stab2 = ctab_l[ti % 2], stab_l[ti % 2], ctab2_l[ti % 2], stab2_l[ti % 2]
                kind = g[0]
                if kind in ("copy", "sig", "rope", "ropei"):
                    _, c0, M, dst_fn, scale = g
                    ps = PS[oi[0] % 2]
                    for kk in range(8):
                        k.op("pe", lambda e, ps=ps, kk=kk, c0=c0, M=M: e.matmul(ps.ap[0:M, :], lhsT=Wv[:, kk, c0:c0 + M],
                                                                             rhs=hv[:, kk, :], start=(kk == 0), stop=(kk == 7)),
                             reads=[Wb, hT], writes=[ps])
                    o = ost[oi[0] % 4]
                    if kind == "copy":
                        k.op("act", lambda e, ps=ps, o=o, M=M, scale=scale: e.activation(out=o.ap[0:M, :], in_=ps.ap[0:M, :],
                                                                                        func=AF.Identity, scale=scale),
                             reads=[ps], writes=[o])
                    elif kind == "sig":
                        k.op("act", lambda e, ps=ps, o=o, M=M: e.activation(out=o.ap[0:M, :], in_=ps.ap[0:M, :], func=AF.Sigmoid),
                             reads=[ps], writes=[o])
                    else:
                        ct, st_ = (ctab, stab) if kind == "rope" else (ctab2, stab2)
                        ps2 = PS[2 + oi[0] % 2]
                        for kk in range(8):
                            k.op("pe", lambda e, ps2=ps2, kk=kk, c0=c0, M=M: e.matmul(ps2.ap[0:M, :], lhsT=Wv[:, kk, c0 + M:c0 + 2 * M],
                                                                                   rhs=hv[:, kk, :], start=(kk == 0), stop=(kk == 7)),
                                 reads=[Wb, hT], writes=[ps2])
                        k.op("dve", lambda e, ps=ps, M=M, ct=ct: e.tensor_tensor(out=t1.ap[0:M, :], in0=ps.ap[0:M, :], in1=ct.ap[0:M, :], op=ALU.mult),
                             reads=[ps, ct], writes=[t1])
                        k.op("dve", lambda e, ps2=ps2, M=M, st_=st_: e.tensor_tensor(out=t2.ap[0:M, :], in0=ps2.ap[0:M, :], in1=st_.ap[0:M, :], op=ALU.mult),
                             reads=[ps2, st_], writes=[t2])
                        k.op("pool", lambda e, o=o, M=M: e.tensor_tensor(out=o.ap[0:M, :], in0=t1.ap[0:M, :], in1=t2.ap[0:M, :], op=ALU.add),
                             reads=[t1, t2], writes=[o])
                    for (r0, r1, dd) in dst_fn(tok0):
                        k.dma(dd, o.ap[r0:r1, :], reads=[o])
                    oi[0] += 1
                elif kind == "tm":
                    _, c0, dst_fn = g
                    for sbi in range(4):
                        ps = PS[oi[0] % 2]
                        for kk in range(8):
                            k.op("pe", lambda e, ps=ps, kk=kk, c0=c0, sbi=sbi: e.matmul(ps.ap, lhsT=hv[:, kk, sbi * 128:(sbi + 1) * 128],
                                                                                     rhs=Wv[:, kk, c0:c0 + 512], start=(kk == 0), stop=(kk == 7)),
                                 reads=[Wb, hT], writes=[ps])
                        o = ost[oi[0] % 4]
                        k.op("dve", lambda e, ps=ps, o=o: e.tensor_copy(out=o.ap, in_=ps.ap), reads=[ps], writes=[o])
                        k.dma(dst_fn(tok0 + sbi * 128), o.ap, reads=[o])
                        oi[0] += 1
                elif kind == "wi":
                    _, c0 = g
                    for sbi in range(4):
                        ps = PS[oi[0] % 2]
                        for kk in range(8):
                            k.op("pe", lambda e, ps=ps, kk=kk, c0=c0, sbi=sbi: e.matmul(ps.ap[:, 0:8], lhsT=hv[:, kk, sbi * 128:(sbi + 1) * 128],
                                                                                     rhs=Wv[:, kk, c0:c0 + 8], start=(kk == 0), stop=(kk == 7)),
                                 reads=[Wb, hT], writes=[ps])
                        k.op("dve", lambda e, ps=ps: e.tensor_copy(out=wist.ap, in_=ps.ap[:, 0:8]), reads=[ps], writes=[wist])
                        k.dma(WI[tok0 + sbi * 128:tok0 + (sbi + 1) * 128, :], wist.ap, reads=[wist])
                        oi[0] += 1
        for f_ in norm_part(0):
            f_()
        for ti in range(ntiles):
            gl = groups_fn()
            nx = norm_part(ti + 1) if ti + 1 < ntiles else []
            done = 0
            for gi_, g in enumerate(gl):
                do_group(g, ti)
                want = ((gi_ + 1) * len(nx)) // len(gl)
                while done < want:
                    nx[done]()
                    done += 1
            while done < len(nx):
                nx[done]()
                done += 1
        k.barrier()
        A.reset(m)

    def groups_k():
        gs = []
        for p in range(4):
            gs.append(("rope", p * 256, 128, (lambda t0, p=p: [(0, 64, KA_T[2 * p, :, t0:t0 + 512]), (64, 128, KA_T[2 * p + 1, :, t0:t0 + 512])]), 1.0))
        for p in range(4):
            gs.append(("copy", 1024 + p * 128, 128, (lambda t0, p=p: [(0, 64, KB_T[2 * p, :, t0:t0 + 512]), (64, 128, KB_T[2 * p + 1, :, t0:t0 + 512])]), 1.0))
        gs.append(("rope", 1536, 128, (lambda t0: [(0, 64, KI_T[:, t0:t0 + 512])]), 1.0))
        gs.append(("tm", 1792, (lambda r0: VA[r0:r0 + 128, :])))
        gs.append(("tm", 2304, (lambda r0: VB[128 + r0:128 + r0 + 128, :])))
        return gs

    zt = B(512, BF16, "zt")
    k.op("pool", lambda e: e.memset(zt.ap, 0.0), writes=[zt])
    k.dma(VB[0:128, :], zt.ap, reads=[zt])
    proj_phase(xall, KT_RUN, WK, NK, groups_k, cosK, sinK)

    m = A.mark()
    va_ = [B(512, BF16, "dv_a%d" % i) for i in range(2)]
    vb_ = [B(512, BF16, "dv_b%d" % i) for i in range(2)]
    vo_ = [B(512, BF16, "dv_o%d" % i) for i in range(2)]
    for j in range(KT_RUN * 4):
        a_, b_, o_ = va_[j % 2], vb_[j % 2], vo_[j % 2]
        k.dma(a_.ap, VB[128 + j * 128 - 1:128 + j * 128 + 127, :], writes=[a_])
        k.dma(b_.ap, VB[128 + j * 128:128 + (j + 1) * 128, :], writes=[b_])
        k.op("dve", lambda e, a_=a_, b_=b_, o_=o_: e.tensor_tensor(out=o_.ap, in0=a_.ap, in1=b_.ap, op=ALU.subtract),
             reads=[a_, b_], writes=[o_])
        k.dma(DVB[j * 128:(j + 1) * 128, :], o_.ap, reads=[o_])
    k.barrier()
    A.reset(m)

    def groups_q():
        gs = []
        for p in range(4):
            gs.append(("rope", p * 256, 128, (lambda t0, p=p: [(0, 64, QA_T[2 * p, :, t0:t0 + 512]), (64, 128, QA_T[2 * p + 1, :, t0:t0 + 512])]), 1.0))
        for p in range(4):
            gs.append(("copy", 1024 + p * 128, 128, (lambda t0, p=p: [(0, 64, QB_T[2 * p, :, t0:t0 + 512]), (64, 128, QB_T[2 * p + 1, :, t0:t0 + 512])]), 0.125))
        for p in range(4):
            gs.append(("ropei", 1536 + p * 256, 128, (lambda t0, p=p: [(0, 64, QI_T[2 * p, :, t0:t0 + 512]), (64, 128, QI_T[2 * p + 1, :, t0:t0 + 512])]), 1.0))
        for c in range(8):
            gs.append(("sig", 2560 + c * 128, 128, (lambda t0, c=c: [(0, 128, GA_T[c, :, t0:t0 + 512])]), 1.0))
        for c in range(8):
            gs.append(("sig", 3584 + c * 128, 128, (lambda t0, c=c: [(0, 128, GB_T[c, :, t0:t0 + 512])]), 1.0))
        gs.append(("wi", 4608))
        return gs

    proj_phase(xown, QT_RUN, WQ, NQ, groups_q, cosQ, sinQ, cosI, sinI)

    m = A.mark()
    kiT = B(L, BF16, "kiT")
    k.op("pool", lambda e: e.memset(kiT.ap, 0.0), writes=[kiT])
    k.dma(kiT.ap[0:64, :], KI_T[:, :], writes=[kiT])
    NT_MAX = 16
    Itile = [B(512, F32, "I%d" % i) for i in range(NT_MAX)]
    I0 = Itile[0].ap
    Mn1 = [B(512, BF16, "Mn%d" % i) for i in range(NT_MAX)]
    cjunk = B(L // 2, BF16, "cjunk")
    ajunk = B(L // 2, BF16, "ajunk")
    acnt = B(2, F32, "acnt")
    nmid = B(2, F32, "nmid")
    itmp = [B(512, F32, "itmp%d" % i) for i in range(2)]
    qaT2 = [B(8 * 128, BF16, "qaT%d" % i) for i in range(2)]
    qbT2 = [B(8 * 128, BF16, "qbT%d" % i) for i in range(2)]
    qiT = B(8 * 128, BF16, "qiT")
    wi_t = B(8, F32, "wi_t")
    bst = B(16, F32, "bst")
    kaT = [B(8 * 512, BF16, "kaT%d" % i) for i in range(2)]
    kbT = [B(8 * 512, BF16, "kbT%d" % i) for i in range(2)]
    vat = [B(4 * 512, BF16, "vat%d" % i) for i in range(2)]
    dvt = [B(4 * 512, BF16, "dvt%d" % i) for i in range(2)]
    vlast = B(512, BF16, "vlast")
    zt512 = B(512, BF16, "zt512")
    Pb = [B(512, BF16, "Pb%d" % i) for i in range(6)]
    gam = [B(512, F32, "gam%d" % i) for i in range(6)]
    Hb = [B(512, BF16, "Hb%d" % i) for i in range(6)]
    PT = [B(512, BF16, "PT%d" % i) for i in range(4)]
    rs2 = [B(8 * NT_MAX, F32, "rs%d" % i) for i in range(2)]
    rst = B(16, F32, "rst")
    oab = B(1024, BF16, "oab")
    oabT = [B(1024, BF16, "oabT%d" % i) for i in range(2)]
    for zb in qaT2 + qbT2 + [qiT, zt512] + kaT + kbT:
        k.op("pool", lambda e, zb=zb: e.memset(zb.ap, 0.0), writes=[zb])
    S_ps = [PS[0], PS[1], PS[2], PS[3]]
    T_ps = [PS[4], PS[5]]
    OA_ps = PS[6]
    OB_ps = PS[7]
    qiv = qiT.ap.rearrange("p (h t) -> p h t", h=8)
    cnt = {"s": 0, "t": 0, "cp": 0, "pb": 0, "gm": 0, "kv": 0}

    def tiles_of(i):
        NS = 2 * i + 2
        tiles = []
        s0 = 0
        while s0 < NS:
            nb = min(4, NS - s0)
            tiles.append((s0, nb))
            s0 += nb
        return NS, tiles

    def idx_thunks(i):
        par = i % 2
        NS, tiles = tiles_of(i)
        nt = len(tiles)
        t0 = i * 128
        Mn = Mn1
        th = []

        def loads():
            k.dma(qiT.ap[0:64, :].rearrange("p (h t) -> p h t", h=8), QI_T[:, :, t0:t0 + 128].rearrange("h d t -> d h t"), writes=[qiT])
            k.dma(wi_t.ap, WI[t0:t0 + 128, :], writes=[wi_t])
        th.append(loads)
        for ti, (sb0, nb) in enumerate(tiles):
            W = nb * 128
            It = Itile[ti]
            for h in range(H):
                def f(ti=ti, sb0=sb0, W=W, It=It, h=h):
                    ps = S_ps[cnt["s"] % 4]
                    cnt["s"] += 1
                    k.op("pe", lambda e: e.matmul(ps.ap[:, 0:W], lhsT=qiv[:, h, :], rhs=kiT.ap[:, sb0 * 128:sb0 * 128 + W], start=True, stop=True),
                         reads=[qiT, kiT], writes=[ps])
                    if h == 0:
                        k.op("dve", lambda e: e.tensor_scalar(out=It.ap[:, 0:W], in0=ps.ap[:, 0:W], scalar1=0.0, scalar2=wi_t.ap[:, h:h + 1],
                                                              op0=ALU.max, op1=ALU.mult), reads=[ps, wi_t], writes=[It])
                    else:
                        tm = itmp[h % 2]
                        k.op("dve", lambda e: e.tensor_scalar(out=tm.ap[:, 0:W], in0=ps.ap[:, 0:W], scalar1=0.0, scalar2=wi_t.ap[:, h:h + 1],
                                                              op0=ALU.max, op1=ALU.mult), reads=[ps, wi_t], writes=[tm])
                        k.op("pool", lambda e: e.tensor_tensor(out=It.ap[:, 0:W], in0=It.ap[:, 0:W], in1=tm.ap[:, 0:W], op=ALU.add),
                             reads=[tm, It], writes=[It])
                th.append(f)
        used = Itile[:nt]
        NW = NS * 128
        Ifv = A.t[:, _off(I0):_off(I0) + NW]
        Isp = A.t[:, _off(I0) + NW - 256:_off(I0) + NW]

        def setup():
            k.op("dve", lambda e: e.tensor_reduce(out=bst.ap[:, 0:1], in_=Ifv, axis=AX.X, op=ALU.max), reads=used, writes=[bst])
            k.op("dve", lambda e: e.tensor_reduce(out=bst.ap[:, 1:2], in_=Ifv, axis=AX.X, op=ALU.min), reads=used, writes=[bst])
            k.op("dve", lambda e: e.tensor_tensor(out=bst.ap[:, 2:3], in0=bst.ap[:, 0:1], in1=bst.ap[:, 1:2], op=ALU.subtract),
                 reads=[bst], writes=[bst])
            k.op("dve", lambda e: e.tensor_tensor(out=Isp, in0=Isp, in1=maskI[par], op=ALU.add), reads=[used[-1], cst], writes=[used[-1]])
        th.append(setup)
        NH = NW // 2
        thr_c = 255.5 - 0.5 * (NW - NH)
        IfA = A.t[:, _off(I0):_off(I0) + NH]
        IfB = A.t[:, _off(I0) + NH:_off(I0) + NW]
        for it in range(NBIS):
            def g(it=it):
                stepc = 0.5 ** (it + 1)
                k.op("dve", lambda e: e.tensor_scalar(out=bst.ap[:, 3:4], in0=bst.ap[:, 2:3], scalar1=stepc, scalar2=None, op0=ALU.mult),
                     reads=[bst], writes=[bst])
                k.op("dve", lambda e: e.tensor_tensor(out=bst.ap[:, 4:5], in0=bst.ap[:, 1:2], in1=bst.ap[:, 3:4], op=ALU.add),
                     reads=[bst], writes=[bst])
                k.op("dve", lambda e: e.tensor_scalar(out=nmid.ap[:, 0:1], in0=bst.ap[:, 4:5], scalar1=-1.0, scalar2=None, op0=ALU.mult),
                     reads=[bst], writes=[nmid])
                k.op("act", lambda e: e.activation(out=ajunk.ap[:, 0:NW - NH], in_=IfB, func=AF.Sign, bias=nmid.ap[:, 0:1], scale=1.0,
                                                   accum_out=acnt.ap[:, 0:1]),
                     reads=used + [nmid], writes=[ajunk, acnt])
                k.op("dve", lambda e: e.tensor_scalar(out=cjunk.ap[:, 0:NH], in0=IfA, scalar1=bst.ap[:, 4:5], scalar2=0.0,
                                                      op0=ALU.is_ge, op1=ALU.add, accum_out=bst.ap[:, 5:6]),
                     reads=used + [bst], writes=[cjunk, bst])
                k.op("dve", lambda e: e.scalar_tensor_tensor(out=bst.ap[:, 7:8], in0=acnt.ap[:, 0:1], scalar=0.5, in1=bst.ap[:, 5:6],
                                                             op0=ALU.mult, op1=ALU.add), reads=[acnt, bst], writes=[bst])
                k.op("dve", lambda e: e.tensor_scalar(out=bst.ap[:, 6:7], in0=bst.ap[:, 7:8], scalar1=thr_c, scalar2=bst.ap[:, 3:4],
                                                      op0=ALU.is_ge, op1=ALU.mult), reads=[bst], writes=[bst])
                k.op("dve", lambda e: e.tensor_tensor(out=bst.ap[:, 1:2], in0=bst.ap[:, 1:2], in1=bst.ap[:, 6:7], op=ALU.add),
                     reads=[bst], writes=[bst])
            th.append(g)
        for ti, (sb0, nb) in enumerate(tiles):
            def mk(ti=ti, W=nb * 128):
                k.op("dve", lambda e: e.tensor_scalar(out=Mn[ti].ap[:, 0:W], in0=Itile[ti].ap[:, 0:W], scalar1=bst.ap[:, 1:2], scalar2=NEG,
                                                      op0=ALU.is_lt, op1=ALU.mult), reads=[Itile[ti], bst], writes=[Mn[ti]])
            th.append(mk)
        return th

    def att_thunks(i):
        par = i % 2
        NS, tiles = tiles_of(i)
        nt = len(tiles)
        t0 = i * 128
        Mn = Mn1
        qaT, qbT = qaT2[i % 2], qbT2[i % 2]
        qav = qaT.ap.rearrange("p (h t) -> p h t", h=8)
        qbv = qbT.ap.rearrange("p (h t) -> p h t", h=8)
        rs = rs2[i % 2]
        rsv = rs.ap.rearrange("p (h t) -> p h t", h=8)
        th = []

        def prologue():
            k.dma(qaT.ap[0:64, :].rearrange("p (h t) -> p h t", h=8), QA_T[:, :, t0:t0 + 128].rearrange("h d t -> d h t"), writes=[qaT])
            k.dma(qbT.ap[0:64, :].rearrange("p (h t) -> p h t", h=8), QB_T[:, :, t0:t0 + 128].rearrange("h d t -> d h t"), writes=[qbT])
            k.dma(vlast.ap, VB[128 + (NS - 1) * 128:128 + NS * 128, :], writes=[vlast])
            k.op("pe", lambda e: e.matmul(OA_ps.ap, lhsT=onehot_b, rhs=zt512.ap, start=True, stop=False), reads=[cstb, zt512], writes=[OA_ps])
            k.op("pe", lambda e: e.matmul(OB_ps.ap, lhsT=onehot_b, rhs=vlast.ap, start=True, stop=False), reads=[cstb, vlast], writes=[OB_ps])
        th.append(prologue)
        wts = [0.0]
        chains = []
        tinfo = {}
        for oi_, ti in enumerate(range(nt - 1, -1, -1)):
            for br in (("dsa", "sb") if (oi_ + i) % 2 == 0 else ("sb", "dsa")):
                for h in range(H):
                    chains.append({"oi": oi_, "ti": ti, "br": br, "h": h})
        carry = [False] * H

        def stage1(c):
            oi_, ti, br, h = c["oi"], c["ti"], c["br"], c["h"]
            sb0, nb = tiles[ti]
            W = nb * 128
            if ti not in tinfo:
                sl = cnt["kv"] % 2
                cnt["kv"] += 1
                ka, kb, va, dv = kaT[sl], kbT[sl], vat[sl], dvt[sl]
                k.dma(ka.ap[0:64, :].rearrange("p (h s) -> p h s", h=8)[:, :, 0:W],
                      KA_T[:, :, sb0 * 128:sb0 * 128 + W].rearrange("h d s -> d h s"), writes=[ka])
                k.dma(va.ap.rearrange("p (j c) -> p j c", j=4)[:, 0:nb, :],
                      VA[sb0 * 128:sb0 * 128 + W, :].rearrange("(j p) c -> p j c", p=128), writes=[va])
                k.dma(kb.ap[0:64, :].rearrange("p (h s) -> p h s", h=8)[:, :, 0:W],
                      KB_T[:, :, sb0 * 128:sb0 * 128 + W].rearrange("h d s -> d h s"), writes=[kb])
                k.dma(dv.ap.rearrange("p (j c) -> p j c", j=4)[:, 0:nb, :],
                      DVB[sb0 * 128:sb0 * 128 + W, :].rearrange("(j p) c -> p j c", p=128), writes=[dv])
                tinfo[ti] = (ka, kb, va, dv)
            ka, kb, va, dv = tinfo[ti]
            kav = ka.ap.rearrange("p (h s) -> p h s", h=8)
            kbv = kb.ap.rearrange("p (h s) -> p h s", h=8)
            first = (oi_ == 0)
            ps = S_ps[cnt["s"] % 4]
            cnt["s"] += 1
            c["nb"] = nb
            if br == "dsa":
                k.op("pe", lambda e: e.matmul(ps.ap[:, 0:W], lhsT=qav[:, h, :], rhs=kav[:, h, 0:W], start=True, stop=False),
                     reads=[qaT, ka], writes=[ps])
                k.op("pe", lambda e: e.matmul(ps.ap[:, 0:W], lhsT=ident_b, rhs=Mn[ti].ap[:, 0:W], start=False, stop=True),
                     reads=[cstb, Mn[ti]], writes=[ps])
                pb = Pb[cnt["pb"] % 6]
                cnt["pb"] += 1
                k.op("act", lambda e: e.activation(out=pb.ap[:, 0:W], in_=ps.ap[:, 0:W], func=AF.Exp, accum_out=rsv[:, h, ti:ti + 1]),
                     reads=[ps], writes=[pb, rs])
                c["src"] = pb
                c["O"] = OA_ps
                c["V"] = va
            else:
                k.op("pe", lambda e: e.matmul(ps.ap[:, 0:W], lhsT=qbv[:, h, :], rhs=kbv[:, h, 0:W], start=True, stop=(not first)),
                     reads=[qbT, kb], writes=[ps])
                if first:
                    k.op("pe", lambda e: e.matmul(ps.ap[:, W - 256:W], lhsT=ident_b, rhs=maskS[par], start=False, stop=True),
                         reads=[cstb], writes=[ps])
                gm = gam[cnt["gm"] % 6]
                hbuf = Hb[cnt["gm"] % 6]
                cnt["gm"] += 1
                k.op("act", lambda e: e.activation(out=gm.ap[:, 0:W], in_=ps.ap[:, 0:W], func=AF.Sigmoid, scale=-1.0),
                     reads=[ps], writes=[gm])
                if not carry[h]:
                    k.op("dve", lambda e: e.tensor_tensor_scan(out=hbuf.ap[:, 0:W][:, ::-1], data0=gm.ap[:, 0:W][:, ::-1],
                                                               data1=gm.ap[:, 0:W][:, ::-1], initial=1.0, op0=ALU.mult, op1=ALU.bypass),
                         reads=[gm], writes=[hbuf])
                else:
                    k.op("dve", lambda e: e.tensor_tensor_scan(out=hbuf.ap[:, 0:W][:, ::-1], data0=gm.ap[:, 0:W][:, ::-1],
                                                               data1=gm.ap[:, 0:W][:, ::-1], initial=rst.ap[:, 8 + h:9 + h],
                                                               op0=ALU.mult, op1=ALU.bypass),
                         reads=[gm, rst], writes=[hbuf])
                if oi_ != nt - 1:
                    k.op("dve", lambda e: e.tensor_copy(out=rst.ap[:, 8 + h:9 + h], in_=hbuf.ap[:, 0:1]), reads=[hbuf], writes=[rst])
                    carry[h] = True
                c["src"] = hbuf
                c["O"] = OB_ps
                c["V"] = dv

        def stage2(c):
            nb, src = c["nb"], c["src"]
            W = nb * 128
            tp = T_ps[cnt["t"] % 2]
            pt = PT[cnt["t"] % 4]
            cnt["t"] += 1
            for j in range(nb):
                k.op("pe", lambda e, j=j: e.matmul(tp.ap[:, j * 128:(j + 1) * 128], lhsT=src.ap[:, j * 128:(j + 1) * 128], rhs=ident_b,
                                                   start=True, stop=True), reads=[src, cstb], writes=[tp])
            if cnt["cp"] % 8 == 0:
                k.op("dve", lambda e: e.tensor_copy(out=pt.ap[:, 0:W], in_=tp.ap[:, 0:W]), reads=[tp], writes=[pt])
            else:
                k.op("act", lambda e: e.activation(out=pt.ap[:, 0:W], in_=tp.ap[:, 0:W], func=AF.Identity), reads=[tp], writes=[pt])
            cnt["cp"] += 1
            c["pt"] = pt

        def stage3(c):
            nb, pt, O_ps, vt, h = c["nb"], c["pt"], c["O"], c["V"], c["h"]
            vv = vt.ap.rearrange("p (j c) -> p j c", j=4)
            for j in range(nb):
                k.op("pe", lambda e, j=j: e.matmul(O_ps.ap[:, h * 64:(h + 1) * 64], lhsT=pt.ap[:, j * 128:(j + 1) * 128],
                                                   rhs=vv[:, j, h * 64:(h + 1) * 64], start=False, stop=False),
                     reads=[pt, vt], writes=[O_ps])

        nch = len(chains)
        for n in range(nch + 4):
            def step(n=n):
                if n < nch:
                    stage1(chains[n])
                if 0 <= n - 2 < nch:
                    stage2(chains[n - 2])
                if 0 <= n - 4 < nch:
                    stage3(chains[n - 4])
            th.append(step)
            wts.append(1.0 if (n < nch and chains[n]["br"] == "dsa") else 0.0)

        def epilogue():
            k.op("dve", lambda e: e.tensor_reduce(out=rst.ap[:, 0:8], in_=rsv[:, :, 0:nt], axis=AX.X, op=ALU.add), reads=[rs], writes=[rst])
            k.op("dve", lambda e: e.reciprocal(out=rst.ap[:, 0:8], in_=rst.ap[:, 0:8]), reads=[rst], writes=[rst])
            for h in range(H):
                k.op("dve", lambda e, h=h: e.tensor_scalar(out=oab.ap[:, h * 64:(h + 1) * 64], in0=OA_ps.ap[:, h * 64:(h + 1) * 64],
                                                         scalar1=rst.ap[:, h:h + 1], scalar2=None, op0=ALU.mult),
                     reads=[OA_ps, rst], writes=[oab])
            k.op("act", lambda e: e.activation(out=oab.ap[:, 512:1024], in_=OB_ps.ap, func=AF.Identity), reads=[OB_ps], writes=[oab])
            ot = oabT[i % 2]
            otv = ot.ap.rearrange("p (c t) -> p c t", c=8)
            for half in range(2):
                tp = T_ps[cnt["t"] % 2]
                cnt["t"] += 1
                for q in range(4):
                    c_ = half * 4 + q
                    k.op("pe", lambda e, tp=tp, q=q, c_=c_: e.matmul(tp.ap[:, q * 128:(q + 1) * 128], lhsT=oab.ap[:, c_ * 128:(c_ + 1) * 128],
                                                                    rhs=ident_b, start=True, stop=True), reads=[oab, cstb], writes=[tp])
                k.op("act", lambda e, tp=tp, half=half: e.activation(out=ot.ap[:, half * 512:(half + 1) * 512], in_=tp.ap, func=AF.Identity),
                     reads=[tp], writes=[ot])
            k.dma(OAB_T[:, :, t0:t0 + 128].rearrange("c p t -> p c t"), otv, reads=[ot])
        th.append(epilogue)
        wts.append(0.0)
        return th, wts

    def split_idx(i):
        th = idx_thunks(i)
        ntl = len(tiles_of(i)[1])
        return th[:-ntl], th[-ntl:]

    main0, tail0 = split_idx(0)
    for f_ in main0 + tail0:
        f_()
    for i in range(NQB_RUN):
        att, wts = att_thunks(i)
        idx, tail = split_idx(i + 1) if i + 1 < NQB_RUN else ([], [])
        na, ni_ = len(att), len(idx)
        wtot = sum(wts)
        wacc = 0.0
        done = 0
        for a_i, f_ in enumerate(att):
            f_()
            wacc += wts[a_i]
            want = int(round(wacc * ni_ / wtot))
            while done < want:
                idx[done]()
                done += 1
        while done < ni_:
            idx[done]()
            done += 1
        for f_ in tail:
            f_()
    k.barrier()
    A.reset(m)

    m = A.mark()
    stage = [B(1024, F32, "wstage%d" % i) for i in range(2)]
    Wup = B(8 * D, BF16, "Wup")
    Wo = B(8 * D, BF16, "Wo")
    load_weight_bf16(Wup, w_up, 8, D, stage)
    load_weight_bf16(Wo, w_out, 8, D, stage)
    Wupv = Wup.ap.rearrange("p (k n) -> p k n", k=8)
    Wov = Wo.ap.rearrange("p (k n) -> p k n", k=8)
    oT = B(8 * 512, BF16, "oT")
    gaT = B(8 * 512, BF16, "gaT")
    gbT = B(8 * 512, BF16, "gbT")
    mg = B(8 * 512, BF16, "mg")
    oTv = oT.ap.rearrange("p (c t) -> p c t", c=8)
    gav = gaT.ap.rearrange("p (c t) -> p c t", c=8)
    gbv = gbT.ap.rearrange("p (c t) -> p c t", c=8)
    mgv = mg.ap.rearrange("p (c t) -> p c t", c=8)
    u1 = [B(512, F32, "u1_%d" % i) for i in range(2)]
    u2 = [B(512, F32, "u2_%d" % i) for i in range(2)]
    xs = [B(D, F32, "xs%d" % i) for i in range(2)]
    x1s = [B(D, F32, "x1s%d" % i) for i in range(2)]
    junk = B(512, BF16, "junk")
    stat = B(8, F32, "stat")

    def rms_epilogue(psy, xin, xout, Abc, stat):
        for n in range(2):
            k.op("act", lambda e, n=n: e.activation(out=junk.ap, in_=psy[n].ap, func=AF.Square, accum_out=stat.ap[:, n:n + 1]),
                 reads=[psy[n]], writes=[junk, stat])
        k.op("dve", lambda e: e.tensor_tensor(out=stat.ap[:, 2:3], in0=stat.ap[:, 0:1], in1=stat.ap[:, 1:2], op=ALU.add), reads=[stat], writes=[stat])
        k.op("act", lambda e: e.activation(out=stat.ap[:, 3:4], in_=stat.ap[:, 2:3], func=AF.Sqrt, scale=1.0 / D, bias=cols.ap[:, 32:33]),
             reads=[stat, cols], writes=[stat])
        k.op("dve", lambda e: e.reciprocal(out=stat.ap[:, 4:5], in_=stat.ap[:, 3:4]), reads=[stat], writes=[stat])
        for n in range(2):
            k.op("dve", lambda e, n=n: e.scalar_tensor_tensor(out=xout.ap[:, n * 512:(n + 1) * 512], in0=psy[n].ap, scalar=stat.ap[:, 4:5],
                                                             in1=Abc.ap[:, n * 512:(n + 1) * 512], op0=ALU.mult, op1=ALU.mult),
                 reads=[psy[n], stat, Abc], writes=[xout])
        k.op("pool", lambda e: e.tensor_tensor(out=xout.ap, in0=xout.ap, in1=xin.ap, op=ALU.add), reads=[xout, xin], writes=[xout])

    ci = 0
    for ti in range(QT_RUN):
        tok0 = ti * 512
        k.dma(oTv, OAB_T[:, :, tok0:tok0 + 512].rearrange("c p t -> p c t"), writes=[oT])
        k.dma(gav, GA_T[:, :, tok0:tok0 + 512].rearrange("c p t -> p c t"), writes=[gaT])
        k.dma(gbv, GB_T[:, :, tok0:tok0 + 512].rearrange("c p t -> p c t"), writes=[gbT])
        for c in range(8):
            pa, pb_ = PS[(ci % 2) * 2], PS[(ci % 2) * 2 + 1]
            a1, a2 = u1[ci % 2], u2[ci % 2]
            ci += 1
            for kk in range(4):
                k.op("pe", lambda e, pa=pa, kk=kk, c=c: e.matmul(pa.ap, lhsT=Wupv[:, kk, c * 128:(c + 1) * 128], rhs=oTv[:, kk, :],
                                                                start=(kk == 0), stop=(kk == 3)), reads=[Wup, oT], writes=[pa])
            for kk in range(4):
                k.op("pe", lambda e, pb_=pb_, kk=kk, c=c: e.matmul(pb_.ap, lhsT=Wupv[:, 4 + kk, c * 128:(c + 1) * 128], rhs=oTv[:, 4 + kk, :],
                                                                  start=(kk == 0), stop=(kk == 3)), reads=[Wup, oT], writes=[pb_])
            k.op("dve", lambda e, pa=pa, a1=a1, c=c: e.tensor_tensor(out=a1.ap, in0=pa.ap, in1=gav[:, c, :], op=ALU.mult), reads=[pa, gaT], writes=[a1])
            k.op("dve", lambda e, pb_=pb_, a2=a2, c=c: e.tensor_tensor(out=a2.ap, in0=pb_.ap, in1=gbv[:, c, :], op=ALU.mult), reads=[pb_, gbT], writes=[a2])
            k.op("pool", lambda e, a1=a1, a2=a2, c=c: e.tensor_tensor(out=mgv[:, c, :], in0=a1.ap, in1=a2.ap, op=ALU.add), reads=[a1, a2], writes=[mg])
        for sbi in range(4):
            psy = [PS[4 + (sbi % 2) * 2], PS[5 + (sbi % 2) * 2]]
            xin, xo = xs[sbi % 2], x1s[sbi % 2]
            k.dma(xin.ap, xown[tok0 + sbi * 128:tok0 + (sbi + 1) * 128, :], writes=[xin])
            for n in range(2):
                for c in range(8):
                    k.op("pe", lambda e, n=n, c=c, sbi=sbi, psy=psy: e.matmul(psy[n].ap, lhsT=mgv[:, c, sbi * 128:(sbi + 1) * 128],
                                                                             rhs=Wov[:, c, n * 512:(n + 1) * 512], start=(c == 0), stop=(c == 7)),
                         reads=[mg, Wo], writes=[psy[n]])
            rms_epilogue(psy, xin, xo, A1bc, stat)
            k.dma(X1[tok0 + sbi * 128:tok0 + (sbi + 1) * 128, :], xo.ap, reads=[xo])
    k.barrier()
    A.reset(m)

    m = A.mark()
    stage = [B(1024, F32, "wstage%d" % i) for i in range(2)]
    W1 = B(8 * DFF, BF16, "W1")
    W2 = B(32 * D, BF16, "W2")
    load_weight_bf16(W1, w_ff1, 8, DFF, stage)
    load_weight_bf16(W2, w_ff2, 32, D, stage)
    W1v = W1.ap.rearrange("p (k n) -> p k n", k=8)
    W2v = W2.ap.rearrange("p (k n) -> p k n", k=32)
    TT = 256
    x1t = [B(D, F32, "x1t%d" % i) for i in range(2)]
    junk2 = B(D, BF16, "junk2")
    junk = B(512, BF16, "junk")
    stat2 = B(4, F32, "stat2")
    stat = B(8, F32, "stat")
    hn = B(D, BF16, "hn")
    h2T = B(8 * TT, BF16, "h2T")
    h2v = h2T.ap.rearrange("p (k n) -> p k n", k=8)
    uT = B(32 * TT, BF16, "uT")
    uTv = uT.ap.rearrange("p (f t) -> p f t", f=32)
    rl = [B(TT, BF16, "rl%d" % i) for i in range(2)]
    outs = [B(D, F32, "outs%d" % i) for i in range(2)]
    for ti in range(QT_RUN * 2):
        tok0 = ti * TT
        for sbi in range(2):
            x_ = x1t[sbi]
            k.dma(x_.ap, X1[tok0 + sbi * 128:tok0 + (sbi + 1) * 128, :], writes=[x_])
            norm_transpose(x_, h2T, 0, sbi, TT, junk2, stat2, hn, 2)
        for f in range(32):
            ps = PS[f % 2]
            for kk in range(8):
                k.op("pe", lambda e, ps=ps, kk=kk, f=f: e.matmul(ps.ap[:, 0:TT], lhsT=W1v[:, kk, f * 128:(f + 1) * 128], rhs=h2v[:, kk, :],
                                                                start=(kk == 0), stop=(kk == 7)), reads=[W1, h2T], writes=[ps])
            r_ = rl[f % 2]
            k.op("act", lambda e, ps=ps, r_=r_: e.activation(out=r_.ap, in_=ps.ap[:, 0:TT], func=AF.Relu), reads=[ps], writes=[r_])
            k.op("dve", lambda e, r_=r_, f=f: e.tensor_tensor(out=uTv[:, f, :], in0=r_.ap, in1=r_.ap, op=ALU.mult), reads=[r_], writes=[uT])
        for sbi in range(2):
            psy = [PS[2 + sbi * 2], PS[3 + sbi * 2]]
            for n in range(2):
                for f in range(32):
                    k.op("pe", lambda e, n=n, f=f, sbi=sbi, psy=psy: e.matmul(psy[n].ap, lhsT=uTv[:, f, sbi * 128:(sbi + 1) * 128],
                                                                             rhs=W2v[:, f, n * 512:(n + 1) * 512], start=(f == 0), stop=(f == 31)),
                         reads=[uT, W2], writes=[psy[n]])
            o_ = outs[sbi]
            rms_epilogue(psy, x1t[sbi], o_, A2bc, stat)
            k.dma(out_own[tok0 + sbi * 128:tok0 + (sbi + 1) * 128, :], o_.ap, reads=[o_])
    k.barrier()
    A.reset(m)
    k.replay()
    return nc


def _off(ap):
    return ap.offset


def _own_blocks(r):
    return [2 * i + ((i % 2) if r == 0 else (1 - i % 2)) for i in range(NQB)]


def _rope_tables(pos):
    inv = (500000.0 ** (-(np.arange(0, 16, 2, dtype=np.float32) / 16.0))).astype(np.float32)
    ang = pos.astype(np.float32)[:, None] * inv[None, :]
    c = np.cos(ang).astype(np.float32)
    s = np.sin(ang).astype(np.float32)
    T = pos.shape[0]
    cf = np.ones((64, T), np.float32)
    sf = np.zeros((64, T), np.float32)
    cf[0:8] = c.T
    cf[8:16] = c.T
    sf[0:8] = -s.T
    sf[8:16] = s.T
    return cf, sf


_PERM = np.concatenate([np.arange(8, 16), np.arange(0, 8), np.arange(16, 64)])


def _consts(r):
    cst = np.zeros((128, 1536), np.float32)
    cst[:, 0:128] = np.eye(128, dtype=np.float32)
    cst[127, 128:256] = 1.0
    tl = np.arange(128)[:, None]
    sl = np.arange(128)[None, :]
    BIG = 1e30
    diag_incl = np.where(sl <= tl, 0.0, 1.0).astype(np.float32)
    diag_strict = np.where(sl < tl, 0.0, 1.0).astype(np.float32)
    full = np.ones((128, 128), np.float32)
    none = np.zeros((128, 128), np.float32)
    for par in range(2):
        off = par if r == 0 else 1 - par
        if off == 0:
            mi = np.concatenate([diag_incl, full], axis=1)
            ms = np.concatenate([diag_strict, full], axis=1)
        else:
            mi = np.concatenate([none, diag_incl], axis=1)
            ms = np.concatenate([none, diag_strict], axis=1)
        cst[:, 256 + par * 256:512 + par * 256] = mi * (-BIG)
        cst[:, 768 + par * 256:1024 + par * 256] = ms * NEG
    cst[:, 1280:1408] = 1.0
    return cst


def kernel(x, c, w_ada, b_ada, g_pre_mix, w_in, w_up_a, w_up_b, w_out, g_post_mix, g_pre_ffn, w_ff1, w_ff2, g_post_ffn):
    debug = bool(os.environ.get("MK_DEBUG"))
    f = np.float32
    x = np.asarray(x, f)
    w_in0 = np.asarray(w_in, f)[0]
    o = np.cumsum([0, 512, 512, 512, 512, 512, 512, 512, 64, 8, 1024, 1024])
    qa_c, ka_c, va_c, qb_c, kb_c, vb_c, qi_c, ki_c, wi_c, ga_c, gb_c = [np.arange(o[j], o[j + 1]) for j in range(11)]

    def rope_pairs(cols, nh):
        out = []
        for p in range(nh // 2):
            h0 = cols[(2 * p) * 64:(2 * p + 1) * 64]
            h1 = cols[(2 * p + 1) * 64:(2 * p + 2) * 64]
            out += [h0, h1, h0[_PERM], h1[_PERM]]
        return np.concatenate(out)

    ki_ext = np.concatenate([ki_c, ki_c, ki_c[_PERM], ki_c[_PERM]])
    colsK = np.concatenate([rope_pairs(ka_c, 8), kb_c, ki_ext, va_c, vb_c])
    colsQ = np.concatenate([rope_pairs(qa_c, 8), qb_c, rope_pairs(qi_c, 8), ga_c, gb_c, wi_c])
    assert colsK.shape[0] == NK and colsQ.shape[0] == NQ
    WK = np.ascontiguousarray(w_in0[:, colsK])
    WQ = np.ascontiguousarray(w_in0[:, colsQ])
    w_up = np.ascontiguousarray(np.concatenate([np.asarray(w_up_a, f)[0], np.asarray(w_up_b, f)[0]], axis=0))
    g_rows = np.ascontiguousarray(np.stack([np.asarray(g_pre_mix, f)[0], np.asarray(g_post_mix, f)[0],
                                            np.asarray(g_pre_ffn, f)[0], np.asarray(g_post_ffn, f)[0]], axis=0))
    cosK, sinK = _rope_tables(np.arange(L))
    in_maps = []
    own_rows = []
    for core in range(8):
        b, r = core // 2, core % 2
        blocks = _own_blocks(r)
        rows = np.concatenate([np.arange(g * 128, (g + 1) * 128) for g in blocks])
        own_rows.append(rows)
        cq, sq = _rope_tables(rows)
        in_maps.append({
            "xall": np.ascontiguousarray(x[b]),
            "xown": np.ascontiguousarray(x[b][rows]),
            "ccol": np.ascontiguousarray(np.asarray(c, f)[b].reshape(8, 128).T),
            "w_ada": np.ascontiguousarray(np.asarray(w_ada, f)[0]),
            "b_ada": np.ascontiguousarray(np.asarray(b_ada, f)[0:1]),
            "g_rows": g_rows,
            "WK": WK, "WQ": WQ, "w_up": w_up,
            "w_out": np.ascontiguousarray(np.asarray(w_out, f)[0]),
            "w_ff1": np.ascontiguousarray(np.asarray(w_ff1, f)[0]),
            "w_ff2": np.ascontiguousarray(np.asarray(w_ff2, f)[0]),
            "cosK": cosK, "sinK": sinK,
            "cosQ": np.ascontiguousarray(cq * 0.125), "sinQ": np.ascontiguousarray(sq * 0.125),
            "cosI": cq, "sinI": sq,
            "consts": _consts(r),
        })
    nc = build_program(debug)
    res = run_bass_kernel_spmd(nc, in_maps, core_ids=list(range(8)))
    out = np.zeros((4, L, D), np.float32)
    for core in range(8):
        out[core // 2][own_rows[core]] = res.results[core]["out_own"]
    if debug:
        kernel._dbg = res.results
    return out
```

```python
import os
import numpy as np
import concourse.bass as bass
import concourse.mybir as mybir
from concourse.bass_utils import run_bass_kernel_spmd

F32 = mybir.dt.float32
BF16 = mybir.dt.bfloat16
AF = mybir.ActivationFunctionType
ALU = mybir.AluOpType
AX = mybir.AxisListType

D = 1024
L = 8192
NB = 64
OWN = 4096
NQB = 32
H = 8
DH = 64
DFF = 4096
EPS = 1e-6
NEG = -30000.0
NBIS = 16
ARENA_F32 = 52000

NK = 2816
NQ = 4616


class Buf:
    __slots__ = ("ap", "lw", "rd", "wsem", "wcnt", "rsem", "rcnt", "name")

    def __init__(self, ap, name=""):
        self.ap = ap
        self.lw = None
        self.rd = []
        self.wsem = None
        self.wcnt = 0
        self.rsem = None
        self.rcnt = 0
        self.name = name


class K:
    ENG = ("pe", "act", "dve", "pool", "sp")

    def __init__(self, nc):
        self.nc = nc
        self.ops = {e: [] for e in self.ENG}
        self.cnt = {e: 0 for e in self.ENG}
        self.waited = {e: {} for e in self.ENG}
        self.dma_keys = []
        self.dma_cnt = {}
        self.nsem = 0
        self.store_key = self.new_dma_key()

    def new_dma_key(self):
        k = "dma%d" % len(self.dma_keys)
        self.dma_keys.append(k)
        self.dma_cnt[k] = 0
        return k

    def _waits(self, eng, toks, same_engine_raw):
        ws = []
        wd = self.waited[eng]
        for t in toks:
            if t is None:
                continue
            key, val = t
            if key == eng and not same_engine_raw:
                continue
            if wd.get(key, 0) >= val:
                continue
            wd[key] = val
            ws.append((key, val))
        return ws

    def op(self, eng, fn, reads=(), writes=(), extra=()):
        toks = list(extra)
        raw = []
        for b in reads:
            if b.lw is not None:
                raw.append(b.lw)
        war = []
        for b in writes:
            if b.lw is not None:
                war.append(b.lw)
            war.extend(b.rd)
        ws = self._waits(eng, [t for t in raw if not (t[0] == eng and eng == "pe")], True)
        ws += self._waits(eng, [t for t in (war + toks) if t[0] != eng], True)
        self.cnt[eng] += 1
        tok = (eng, self.cnt[eng])
        self.ops[eng].append((fn, ws, 1))
        for b in reads:
            b.rd.append(tok)
        for b in writes:
            b.lw = tok
            b.rd = []
        return tok

    def dma(self, out, in_, reads=(), writes=(), key=None):
        eng = "sp"
        toks = []
        for b in reads:
            if b.lw is not None:
                toks.append(b.lw)
        for b in writes:
            if b.lw is not None:
                toks.append(b.lw)
            toks.extend(b.rd)
        ws = self._waits(eng, toks, True)
        if writes:
            b = writes[0]
            if b.wsem is None:
                b.wsem = self.new_dma_key()
            k = b.wsem
        elif reads:
            b = reads[0]
            if b.rsem is None:
                b.rsem = self.new_dma_key()
            k = b.rsem
        else:
            k = self.store_key
        if key is not None:
            k = key
        self.dma_cnt[k] += 16
        tok = (k, self.dma_cnt[k])
        self.ops[eng].append((lambda e, o=out, i=in_: e.dma_start(out=o, in_=i), ws, (k, 16)))
        for b in reads:
            b.rd.append(tok)
        for b in writes:
            b.lw = tok
            b.rd = []
        return tok

    def barrier(self):
        toks = [(e, self.cnt[e]) for e in self.ENG if self.cnt[e] > 0]
        toks += [(k, v) for k, v in self.dma_cnt.items() if v > 0]
        for e in self.ENG:
            ws = self._waits(e, toks, False)
            if ws:
                self.ops[e].append((None, ws, 0))

    def replay(self):
        nc = self.nc
        keys = list(self.ENG) + self.dma_keys
        import contextlib
        with contextlib.ExitStack() as st:
            sems = {k: st.enter_context(nc.semaphore("s_" + k)) for k in keys}
            block = st.enter_context(nc.Block())

            def run(e, name):
                for fn, ws, sig in self.ops[name]:
                    for key, val in ws:
                        e.wait_ge(sems[key], val)
                    if fn is None:
                        continue
                    ins = fn(e)
                    if sig == 1:
                        ins.then_inc(sems[name], 1)
                    elif sig:
                        ins.then_inc(sems[sig[0]], sig[1])

            @block.tensor
            def _(e):
                run(e, "pe")

            @block.scalar
            def _(e):
                run(e, "act")

            @block.vector
            def _(e):
                run(e, "dve")

            @block.gpsimd
            def _(e):
                run(e, "pool")

            @block.sync
            def _(e):
                run(e, "sp")


class Arena:
    def __init__(self, t, nwords):
        self.t = t
        self.n = nwords
        self.off = 0

    def mark(self):
        return self.off

    def reset(self, m):
        self.off = m

    def alloc(self, n, dt=F32, name=""):
        w = n if dt == F32 else (n + 1) // 2
        assert self.off + w <= self.n, ("arena overflow", name, self.off, w, self.n)
        v = self.t[:, self.off:self.off + w]
        self.off += w
        if dt != F32:
            v = v.bitcast(dt)
        return v


def build_program(debug=False):
    NQB_RUN = int(os.environ.get('MK_NQB', NQB))
    KT_RUN = int(os.environ.get('MK_KT', L // 512))
    QT_RUN = int(os.environ.get('MK_QT', OWN // 512))
    nc = bass.Bass("TRN2", target_bir_lowering=False)

    def din(name, shape, dt=F32):
        return nc.dram_tensor(name, list(shape), dt, kind="ExternalInput").ap()

    skind = "ExternalOutput" if debug else "Internal"

    def dscr(name, shape, dt):
        return nc.dram_tensor(name, list(shape), dt, kind=skind).ap()

    xall = din("xall", [L, D])
    xown = din("xown", [OWN, D])
    ccol = din("ccol", [128, 8])
    w_ada = din("w_ada", [D, 6 * D])
    b_ada = din("b_ada", [1, 6 * D])
    g_rows = din("g_rows", [4, D])
    WK = din("WK", [D, NK])
    WQ = din("WQ", [D, NQ])
    w_up = din("w_up", [D, D])
    w_out = din("w_out", [D, D])
    w_ff1 = din("w_ff1", [D, DFF])
    w_ff2 = din("w_ff2", [DFF, D])
    cosK = din("cosK", [64, L])
    sinK = din("sinK", [64, L])
    cosQ = din("cosQ", [64, OWN])
    sinQ = din("sinQ", [64, OWN])
    cosI = din("cosI", [64, OWN])
    sinI = din("sinI", [64, OWN])
    consts = din("consts", [128, 1536])
    out_own = nc.dram_tensor("out_own", [OWN, D], F32, kind="ExternalOutput").ap()

    KA_T = dscr("KA_T", [H, 64, L], BF16)
    KB_T = dscr("KB_T", [H, 64, L], BF16)
    KI_T = dscr("KI_T", [64, L], BF16)
    VA = dscr("VA", [L, 512], BF16)
    VB = dscr("VB", [L + 128, 512], BF16)
    DVB = dscr("DVB", [L, 512], BF16)
    QA_T = dscr("QA_T", [H, 64, OWN], BF16)
    QB_T = dscr("QB_T", [H, 64, OWN], BF16)
    QI_T = dscr("QI_T", [H, 64, OWN], BF16)
    GA_T = dscr("GA_T", [8, 128, OWN], BF16)
    GB_T = dscr("GB_T", [8, 128, OWN], BF16)
    WI = dscr("WI", [OWN, 8], F32)
    OAB_T = dscr("OAB_T", [8, 128, OWN], BF16)
    X1 = dscr("X1", [OWN, D], F32)

    arena_t = nc.alloc_sbuf_tensor("arena", [128, ARENA_F32], F32)
    A = Arena(arena_t, ARENA_F32)
    banks = [nc.alloc_psum_tensor("ps%d" % i, [128, 512], F32) for i in range(8)]
    PS = [Buf(banks[i][:, :], "ps%d" % i) for i in range(8)]
    k = K(nc)

    def B(n, dt=F32, name=""):
        return Buf(A.alloc(n, dt, name), name)

    cst = B(1536, F32, "cst")
    k.dma(cst.ap, consts[:, :], writes=[cst])
    cstb = B(1536, BF16, "cstb")
    k.op("dve", lambda e: e.tensor_copy(out=cstb.ap, in_=cst.ap), reads=[cst], writes=[cstb])
    ident_f = cst.ap[:, 0:128]
    ident_b = cstb.ap[:, 0:128]
    onehot_b = cstb.ap[:, 128:256]
    maskI = [cst.ap[:, 256:512], cst.ap[:, 512:768]]
    maskS = [cstb.ap[:, 768:1024], cstb.ap[:, 1024:1280]]
    ones_f = cst.ap[:, 1280:1408]
    cols = B(33, F32, "cols")
    A1bc = B(1024, F32, "A1bc")
    A2bc = B(1024, F32, "A2bc")

    m0 = A.mark()
    cc = B(8, F32, "cc")
    k.dma(cc.ap, ccol[:, :], writes=[cc])
    sg = B(8, F32, "sg")
    k.op("act", lambda e: e.activation(out=sg.ap, in_=cc.ap, func=AF.Silu), reads=[cc], writes=[sg])
    modr = B(6 * D, F32, "modr")
    bar = B(6 * D, F32, "bar")
    k.dma(bar.ap, b_ada[0:1, :].broadcast_to([128, 6 * D]), writes=[bar])
    gr = B(4 * D, F32, "gr")
    for i in range(4):
        k.dma(gr.ap[:, i * D:(i + 1) * D], g_rows[i:i + 1, :].broadcast_to([128, D]), writes=[gr])
    sgb = B(8 * 128, F32, "sgb")
    sgv = sgb.ap.rearrange("p (k n) -> p k n", k=8)
    for kk in range(8):
        k.op("dve", lambda e, kk=kk: e.tensor_scalar(out=sgv[:, kk, :], in0=ones_f, scalar1=sg.ap[:, kk:kk + 1], scalar2=None, op0=ALU.mult),
             reads=[sg, cst], writes=[sgb])
    wst = [B(512, F32, "wst%d" % i) for i in range(4)]
    ni = 0
    for cchunk in range(12):
        ps = PS[cchunk % 2]
        for kk in range(8):
            w = wst[ni % 4]
            ni += 1
            k.dma(w.ap, w_ada[kk * 128:(kk + 1) * 128, cchunk * 512:(cchunk + 1) * 512], writes=[w])
            k.op("pe", lambda e, ps=ps, w=w, kk=kk: e.matmul(ps.ap, lhsT=sgv[:, kk, :], rhs=w.ap, start=(kk == 0), stop=(kk == 7)),
                 reads=[sgb, w], writes=[ps])
        k.op("dve", lambda e, ps=ps, c=cchunk: e.tensor_tensor(out=modr.ap[:, c * 512:(c + 1) * 512], in0=ps.ap,
                                                              in1=bar.ap[:, c * 512:(c + 1) * 512], op=ALU.add),
             reads=[ps, bar], writes=[modr])
    rows = B(2 * D, F32, "rows")

    def mrow(i):
        return modr.ap[:, i * D:(i + 1) * D]

    k.op("dve", lambda e: e.scalar_tensor_tensor(out=rows.ap[:, 0:D], in0=mrow(1), scalar=1.0, in1=gr.ap[:, 0:D],
                                                 op0=ALU.add, op1=ALU.mult), reads=[modr, gr], writes=[rows])
    k.op("dve", lambda e: e.scalar_tensor_tensor(out=rows.ap[:, D:2 * D], in0=mrow(4), scalar=1.0, in1=gr.ap[:, 2 * D:3 * D],
                                                 op0=ALU.add, op1=ALU.mult), reads=[modr, gr], writes=[rows])
    k.op("dve", lambda e: e.tensor_tensor(out=A1bc.ap, in0=mrow(2), in1=gr.ap[:, D:2 * D], op=ALU.mult),
         reads=[modr, gr], writes=[A1bc])
    k.op("dve", lambda e: e.tensor_tensor(out=A2bc.ap, in0=mrow(5), in1=gr.ap[:, 3 * D:4 * D], op=ALU.mult),
         reads=[modr, gr], writes=[A2bc])
    srcs = [rows.ap[:, 0:D], mrow(0), rows.ap[:, D:2 * D], mrow(3)]
    dtmp = B(128, F32, "dtmp")
    for si in range(4):
        for kk in range(8):
            k.op("dve", lambda e, si=si, kk=kk: e.tensor_tensor(out=dtmp.ap, in0=srcs[si][:, kk * 128:(kk + 1) * 128], in1=ident_f, op=ALU.mult),
                 reads=[rows, modr, cst], writes=[dtmp])
            k.op("dve", lambda e, si=si, kk=kk: e.tensor_reduce(out=cols.ap[:, si * 8 + kk:si * 8 + kk + 1], in_=dtmp.ap, axis=AX.X, op=ALU.add),
                 reads=[dtmp], writes=[cols])
    k.op("pool", lambda e: e.memset(cols.ap[:, 32:33], EPS), reads=[cols], writes=[cols])
    k.barrier()
    A.reset(m0)

    rr = {"cast": 0, "cp": 0}

    def load_weight_bf16(dst, src, nrow_chunks, ncols, stage):
        dv = dst.ap.rearrange("p (k n) -> p k n", k=nrow_chunks)
        for kk in range(nrow_chunks):
            c0 = 0
            while c0 < ncols:
                cw = min(1024, ncols - c0)
                s = stage[rr["cast"] % len(stage)]
                eng = ("dve", "act", "pool")[rr["cast"] % 3]
                rr["cast"] += 1
                k.dma(s.ap[:, 0:cw], src[kk * 128:(kk + 1) * 128, c0:c0 + cw], writes=[s])
                if eng == "act":
                    k.op("act", lambda e, s=s, kk=kk, c0=c0, cw=cw: e.activation(out=dv[:, kk, c0:c0 + cw], in_=s.ap[:, 0:cw], func=AF.Identity),
                         reads=[s], writes=[dst])
                else:
                    k.op(eng, lambda e, s=s, kk=kk, c0=c0, cw=cw: e.tensor_copy(out=dv[:, kk, c0:c0 + cw], in_=s.ap[:, 0:cw]),
                         reads=[s], writes=[dst])
                c0 += cw

    def norm_transpose(xt, hT, col0, sbi, ntok_tile, junk, stat, hn, gi):
        hv = hT.ap.rearrange("p (k n) -> p k n", k=8)
        k.op("act", lambda e: e.activation(out=junk.ap, in_=xt.ap, func=AF.Square, accum_out=stat.ap[:, 0:1]),
             reads=[xt], writes=[junk, stat])
        k.op("act", lambda e: e.activation(out=stat.ap[:, 1:2], in_=stat.ap[:, 0:1], func=AF.Sqrt, scale=1.0 / D, bias=cols.ap[:, 32:33]),
             reads=[stat, cols], writes=[stat])
        k.op("dve", lambda e: e.reciprocal(out=stat.ap[:, 2:3], in_=stat.ap[:, 1:2]), reads=[stat], writes=[stat])
        k.op("act", lambda e: e.activation(out=hn.ap, in_=xt.ap, func=AF.Identity, scale=stat.ap[:, 2:3]),
             reads=[xt, stat], writes=[hn])
        for half in range(2):
            ps = PS[6 + half]
            for q in range(4):
                kk = half * 4 + q
                k.op("pe", lambda e, ps=ps, q=q, kk=kk: e.matmul(ps.ap[:, q * 128:(q + 1) * 128],
                                                                lhsT=hn.ap[:, kk * 128:(kk + 1) * 128], rhs=ident_b,
                                                                start=True, stop=True),
                     reads=[hn, cstb], writes=[ps])
            for q in range(4):
                kk = half * 4 + q
                k.op("act", lambda e, ps=ps, q=q, kk=kk: e.activation(
                    out=hv[:, kk, col0 + sbi * 128:col0 + (sbi + 1) * 128], in_=ps.ap[:, q * 128:(q + 1) * 128],
                    func=AF.Identity, scale=cols.ap[:, gi * 8 + kk:gi * 8 + kk + 1],
                    bias=cols.ap[:, (gi + 1) * 8 + kk:(gi + 1) * 8 + kk + 1]),
                     reads=[ps, cols], writes=[hT])

    def proj_phase(xsrc, ntiles, Wd, ncols, groups_fn, cos_d, sin_d, cosi_d=None, sini_d=None):
        m = A.mark()
        Wb = B(8 * ncols, BF16, "Wb")
        stage = [B(1024, F32, "wstage%d" % i) for i in range(2)]
        load_weight_bf16(Wb, Wd, 8, ncols, stage)
        Wv = Wb.ap.rearrange("p (k n) -> p k n", k=8)
        xt = [B(D, F32, "xt%d" % i) for i in range(2)]
        junk = B(D, BF16, "junk")
        stat = B(4, F32, "stat")
        hn = B(D, BF16, "hn")
        hT_l = [B(8 * 512, BF16, "hT%d" % i) for i in range(2)]
        ctab_l = [B(512, F32, "ctab%d" % i) for i in range(2)]
        stab_l = [B(512, F32, "stab%d" % i) for i in range(2)]
        ctab2_l = [B(512, F32, "ctab2_%d" % i) for i in range(2)]
        stab2_l = [B(512, F32, "stab2_%d" % i) for i in range(2)]
        t1 = B(512, F32, "t1")
        t2 = B(512, F32, "t2")
        ost = [B(512, BF16, "ost%d" % i) for i in range(4)]
        wist = B(8, F32, "wist")
        oi = [0]

        def norm_part(ti):
            tok0 = ti * 512
            hT = hT_l[ti % 2]
            th = []
            for sbi in range(4):
                def f(sbi=sbi):
                    x_ = xt[sbi % 2]
                    k.dma(x_.ap, xsrc[tok0 + sbi * 128:tok0 + (sbi + 1) * 128, :], writes=[x_])
                    norm_transpose(x_, hT, 0, sbi, 512, junk, stat, hn, 0)
                th.append(f)

            def tabs():
                ctab, stab, ctab2, stab2 = ctab_l[ti % 2], stab_l[ti % 2], ctab2_l[ti % 2], stab2_l[ti % 2]
                for hh in range(2):
                    k.dma(ctab.ap[hh * 64:(hh + 1) * 64, :], cos_d[:, tok0:tok0 + 512], writes=[ctab])
                    k.dma(stab.ap[hh * 64:(hh + 1) * 64, :], sin_d[:, tok0:tok0 + 512], writes=[stab])
                if cosi_d is not None:
                    for hh in range(2):
                        k.dma(ctab2.ap[hh * 64:(hh + 1) * 64, :], cosi_d[:, tok0:tok0 + 512], writes=[ctab2])
                        k.dma(stab2.ap[hh * 64:(hh + 1) * 64, :], sini_d[:, tok0:tok0 + 512], writes=[stab2])
            th.append(tabs)
            return th

        if True:
            def do_group(g, ti):
                tok0 = ti * 512
                hT = hT_l[ti % 2]
                hv = hT.ap.rearrange("p (k n) -> p k n", k=8)
                ctab, stab, ctab2, stab2 = ctab_l[ti % 2], stab_l[ti % 2], ctab2_l[ti % 2], stab2_l[ti % 2]
                kind = g[0]
                if kind in ("copy", "sig", "rope", "ropei"):
                    _, c0, M, dst_fn, scale = g
                    ps = PS[oi[0] % 2]
                    for kk in range(8):
                        k.op("pe", lambda e, ps=ps, kk=kk, c0=c0, M=M: e.matmul(ps.ap[0:M, :], lhsT=Wv[:, kk, c0:c0 + M],
                                                                             rhs=hv[:, kk, :], start=(kk == 0), stop=(kk == 7)),
                             reads=[Wb, hT], writes=[ps])
                    o = ost[oi[0] % 4]
                    if kind == "copy":
                        k.op("act", lambda e, ps=ps, o=o, M=M, scale=scale: e.activation(out=o.ap[0:M, :], in_=ps.ap[0:M, :],
                                                                                        func=AF.Identity, scale=scale),
                             reads=[ps], writes=[o])
                    elif kind == "sig":
                        k.op("act", lambda e, ps=ps, o=o, M=M: e.activation(out=o.ap[0:M, :], in_=ps.ap[0:M, :], func=AF.Sigmoid),
                             reads=[ps], writes=[o])
                    else:
                        ct, st_ = (ctab, stab) if kind == "rope" else (ctab2, stab2)
                        ps2 = PS[2 + oi[0] % 2]
                        for kk in range(8):
                            k.op("pe", lambda e, ps2=ps2, kk=kk, c0=c0, M=M: e.matmul(ps2.ap[0:M, :], lhsT=Wv[:, kk, c0 + M:c0 + 2 * M],
                                                                                   rhs=hv[:, kk, :], start=(kk == 0), stop=(kk == 7)),
                                 reads=[Wb, hT], writes=[ps2])
                        k.op("dve", lambda e, ps=ps, M=M, ct=ct: e.tensor_tensor(out=t1.ap[0:M, :], in0=ps.ap[0:M, :], in1=ct.ap[0:M, :], op=ALU.mult),
                             reads=[ps, ct], writes=[t1])
                        k.op("dve", lambda e, ps2=ps2, M=M, st_=st_: e.tensor_tensor(out=t2.ap[0:M, :], in0=ps2.ap[0:M, :], in1=st_.ap[0:M, :], op=ALU.mult),
                             reads=[ps2, st_], writes=[t2])
                        k.op("pool", lambda e, o=o, M=M: e.tensor_tensor(out=o.ap[0:M, :], in0=t1.ap[0:M, :], in1=t2.ap[0:M, :], op=ALU.add),
                             reads=[t1, t2], writes=[o])
                    for (r0, r1, dd) in dst_fn(tok0):
                        k.dma(dd, o.ap[r0:r1, :], reads=[o])
                    oi[0] += 1
                elif kind == "tm":
                    _, c0, dst_fn = g
                    for sbi in range(4):
                        ps = PS[oi[0] % 2]
                        for kk in range(8):
                            k.op("pe", lambda e, ps=ps, kk=kk, c0=c0, sbi=sbi: e.matmul(ps.ap, lhsT=hv[:, kk, sbi * 128:(sbi + 1) * 128],
                                                                                     rhs=Wv[:, kk, c0:c0 + 512], start=(kk == 0), stop=(kk == 7)),
                                 reads=[Wb, hT], writes=[ps])
                        o = ost[oi[0] % 4]
                        k.op("dve", lambda e, ps=ps, o=o: e.tensor_copy(out=o.ap, in_=ps.ap), reads=[ps], writes=[o])
                        k.dma(dst_fn(tok0 + sbi * 128), o.ap, reads=[o])
                        oi[0] += 1
                elif kind == "wi":
                    _, c0 = g
                    for sbi in range(4):
                        ps = PS[oi[0] % 2]
                        for kk in range(8):
                            k.op("pe", lambda e, ps=ps, kk=kk, c0=c0, sbi=sbi: e.matmul(ps.ap[:, 0:8], lhsT=hv[:, kk, sbi * 128:(sbi + 1) * 128],
                                                                                     rhs=Wv[:, kk, c0:c0 + 8], start=(kk == 0), stop=(kk == 7)),
                                 reads=[Wb, hT], writes=[ps])
                        k.op("dve", lambda e, ps=ps: e.tensor_copy(out=wist.ap, in_=ps.ap[:, 0:8]), reads=[ps], writes=[wist])
                        k.dma(WI[tok0 + sbi * 128:tok0 + (sbi + 1) * 128, :], wist.ap, reads=[wist])
                        oi[0] += 1
        for f_ in norm_part(0):
            f_()
        for ti in range(ntiles):
            gl = groups_fn()
            nx = norm_part(ti + 1) if ti + 1 < ntiles else []
            done = 0
            for gi_, g in enumerate(gl):
                do_group(g, ti)
                want = ((gi_ + 1) * len(nx)) // len(gl)
                while done < want:
                    nx[done]()
                    done += 1
            while done < len(nx):
                nx[done]()
                done += 1
        k.barrier()
        A.reset(m)

    def groups_k():
        gs = []
        for p in range(4):
            gs.append(("rope", p * 256, 128, (lambda t0, p=p: [(0, 64, KA_T[2 * p, :, t0:t0 + 512]), (64, 128, KA_T[2 * p + 1, :, t0:t0 + 512])]), 1.0))
        for p in range(4):
            gs.append(("copy", 1024 + p * 128, 128, (lambda t0, p=p: [(0, 64, KB_T[2 * p, :, t0:t0 + 512]), (64, 128, KB_T[2 * p + 1, :, t0:t0 + 512])]), 1.0))
        gs.append(("rope", 1536, 128, (lambda t0: [(0, 64, KI_T[:, t0:t0 + 512])]), 1.0))
        gs.append(("tm", 1792, (lambda r0: VA[r0:r0 + 128, :])))
        gs.append(("tm", 2304, (lambda r0: VB[128 + r0:128 + r0 + 128, :])))
        return gs

    zt = B(512, BF16, "zt")
    k.op("pool", lambda e: e.memset(zt.ap, 0.0), writes=[zt])
    k.dma(VB[0:128, :], zt.ap, reads=[zt])
    proj_phase(xall, KT_RUN, WK, NK, groups_k, cosK, sinK)

    m = A.mark()
    va_ = [B(512, BF16, "dv_a%d" % i) for i in range(2)]
    vb_ = [B(512, BF16, "dv_b%d" % i) for i in range(2)]
    vo_ = [B(512, BF16, "dv_o%d" % i) for i in range(2)]
    for j in range(KT_RUN * 4):
        a_, b_, o_ = va_[j % 2], vb_[j % 2], vo_[j % 2]
        k.dma(a_.ap, VB[128 + j * 128 - 1:128 + j * 128 + 127, :], writes=[a_])
        k.dma(b_.ap, VB[128 + j * 128:128 + (j + 1) * 128, :], writes=[b_])
        k.op("dve", lambda e, a_=a_, b_=b_, o_=o_: e.tensor_tensor(out=o_.ap, in0=a_.ap, in1=b_.ap, op=ALU.subtract),
             reads=[a_, b_], writes=[o_])
        k.dma(DVB[j * 128:(j + 1) * 128, :], o_.ap, reads=[o_])
    k.barrier()
    A.reset(m)

    def groups_q():
        gs = []
        for p in range(4):
            gs.append(("rope", p * 256, 128, (lambda t0, p=p: [(0, 64, QA_T[2 * p, :, t0:t0 + 512]), (64, 128, QA_T[2 * p + 1, :, t0:t0 + 512])]), 1.0))
        for p in range(4):
            gs.append(("copy", 1024 + p * 128, 128, (lambda t0, p=p: [(0, 64, QB_T[2 * p, :, t0:t0 + 512]), (64, 128, QB_T[2 * p + 1, :, t0:t0 + 512])]), 0.125))
        for p in range(4):
            gs.append(("ropei", 1536 + p * 256, 128, (lambda t0, p=p: [(0, 64, QI_T[2 * p, :, t0:t0 + 512]), (64, 128, QI_T[2 * p + 1, :, t0:t0 + 512])]), 1.0))
        for c in range(8):
            gs.append(("sig", 2560 + c * 128, 128, (lambda t0, c=c: [(0, 128, GA_T[c, :, t0:t0 + 512])]), 1.0))
        for c in range(8):
            gs.append(("sig", 3584 + c * 128, 128, (lambda t0, c=c: [(0, 128, GB_T[c, :, t0:t0 + 512])]), 1.0))
        gs.append(("wi", 4608))
        return gs

    proj_phase(xown, QT_RUN, WQ, NQ, groups_q, cosQ, sinQ, cosI, sinI)

    m = A.mark()
    kiT = B(L, BF16, "kiT")
    k.op("pool", lambda e: e.memset(kiT.ap, 0.0), writes=[kiT])
    k.dma(kiT.ap[0:64, :], KI_T[:, :], writes=[kiT])
    NT_MAX = 16
    Itile = [B(512, F32, "I%d" % i) for i in range(NT_MAX)]
    I0 = Itile[0].ap
    Mn1 = [B(512, BF16, "Mn%d" % i) for i in range(NT_MAX)]
    cjunk = B(L // 2, BF16, "cjunk")
    ajunk = B(L // 2, BF16, "ajunk")
    acnt = B(2, F32, "acnt")
    nmid = B(2, F32, "nmid")
    itmp = [B(512, F32, "itmp%d" % i) for i in range(2)]
    qaT2 = [B(8 * 128, BF16, "qaT%d" % i) for i in range(2)]
    qbT2 = [B(8 * 128, BF16, "qbT%d" % i) for i in range(2)]
    qiT = B(8 * 128, BF16, "qiT")
    wi_t = B(8, F32, "wi_t")
    bst = B(16, F32, "bst")
    kaT = [B(8 * 512, BF16, "kaT%d" % i) for i in range(2)]
    kbT = [B(8 * 512, BF16, "kbT%d" % i) for i in range(2)]
    vat = [B(4 * 512, BF16, "vat%d" % i) for i in range(2)]
    dvt = [B(4 * 512, BF16, "dvt%d" % i) for i in range(2)]
    vlast = B(512, BF16, "vlast")
    zt512 = B(512, BF16, "zt512")
    Pb = [B(512, BF16, "Pb%d" % i) for i in range(6)]
    gam = [B(512, F32, "gam%d" % i) for i in range(6)]
    Hb = [B(512, BF16, "Hb%d" % i) for i in range(6)]
    PT = [B(512, BF16, "PT%d" % i) for i in range(4)]
    rs2 = [B(8 * NT_MAX, F32, "rs%d" % i) for i in range(2)]
    rst = B(16, F32, "rst")
    cry_l = [B(2, F32, "cry%d" % i) for i in range(8)]
    oab = B(1024, BF16, "oab")
    oabT = [B(1024, BF16, "oabT%d" % i) for i in range(2)]
    for zb in qaT2 + qbT2 + [qiT, zt512] + kaT + kbT:
        k.op("pool", lambda e, zb=zb: e.memset(zb.ap, 0.0), writes=[zb])
    S_ps = [PS[0], PS[1], PS[2], PS[3]]
    T_ps = [PS[4], PS[5]]
    OA_ps = PS[6]
    OB_ps = PS[7]
    qiv = qiT.ap.rearrange("p (h t) -> p h t", h=8)
    cnt = {"s": 0, "t": 0, "cp": 0, "pb": 0, "gm": 0, "kv": 0}

    def tiles_of(i):
        NS = 2 * i + 2
        tiles = []
        s0 = 0
        while s0 < NS:
            nb = min(4, NS - s0)
            tiles.append((s0, nb))
            s0 += nb
        return NS, tiles

    def idx_thunks(i):
        par = i % 2
        NS, tiles = tiles_of(i)
        nt = len(tiles)
        t0 = i * 128
        Mn = Mn1
        th = []

        def loads():
            k.dma(qiT.ap[0:64, :].rearrange("p (h t) -> p h t", h=8), QI_T[:, :, t0:t0 + 128].rearrange("h d t -> d h t"), writes=[qiT])
            k.dma(wi_t.ap, WI[t0:t0 + 128, :], writes=[wi_t])
        th.append(loads)
        for ti, (sb0, nb) in enumerate(tiles):
            W = nb * 128
            It = Itile[ti]
            for h in range(H):
                def f(ti=ti, sb0=sb0, W=W, It=It, h=h):
                    ps = S_ps[cnt["s"] % 4]
                    cnt["s"] += 1
                    k.op("pe", lambda e: e.matmul(ps.ap[:, 0:W], lhsT=qiv[:, h, :], rhs=kiT.ap[:, sb0 * 128:sb0 * 128 + W], start=True, stop=True),
                         reads=[qiT, kiT], writes=[ps])
                    if h == 0:
                        k.op("dve", lambda e: e.tensor_scalar(out=It.ap[:, 0:W], in0=ps.ap[:, 0:W], scalar1=0.0, scalar2=wi_t.ap[:, h:h + 1],
                                                              op0=ALU.max, op1=ALU.mult), reads=[ps, wi_t], writes=[It])
                    else:
                        tm = itmp[h % 2]
                        k.op("dve", lambda e: e.tensor_scalar(out=tm.ap[:, 0:W], in0=ps.ap[:, 0:W], scalar1=0.0, scalar2=wi_t.ap[:, h:h + 1],
                                                              op0=ALU.max, op1=ALU.mult), reads=[ps, wi_t], writes=[tm])
                        k.op("pool", lambda e: e.tensor_tensor(out=It.ap[:, 0:W], in0=It.ap[:, 0:W], in1=tm.ap[:, 0:W], op=ALU.add),
                             reads=[tm, It], writes=[It])
                th.append(f)
        used = Itile[:nt]
        NW = NS * 128
        Ifv = A.t[:, _off(I0):_off(I0) + NW]
        Isp = A.t[:, _off(I0) + NW - 256:_off(I0) + NW]

        def setup():
            k.op("dve", lambda e: e.tensor_reduce(out=bst.ap[:, 0:1], in_=Ifv, axis=AX.X, op=ALU.max), reads=used, writes=[bst])
            k.op("dve", lambda e: e.tensor_reduce(out=bst.ap[:, 1:2], in_=Ifv, axis=AX.X, op=ALU.min), reads=used, writes=[bst])
            k.op("dve", lambda e: e.tensor_tensor(out=bst.ap[:, 2:3], in0=bst.ap[:, 0:1], in1=bst.ap[:, 1:2], op=ALU.subtract),
                 reads=[bst], writes=[bst])
            k.op("dve", lambda e: e.tensor_tensor(out=Isp, in0=Isp, in1=maskI[par], op=ALU.add), reads=[used[-1], cst], writes=[used[-1]])
        th.append(setup)
        NH = NW // 2
        thr_c = 255.5 - 0.5 * (NW - NH)
        IfA = A.t[:, _off(I0):_off(I0) + NH]
        IfB = A.t[:, _off(I0) + NH:_off(I0) + NW]
        for it in range(NBIS):
            def g(it=it):
                stepc = 0.5 ** (it + 1)
                k.op("dve", lambda e: e.tensor_scalar(out=bst.ap[:, 3:4], in0=bst.ap[:, 2:3], scalar1=stepc, scalar2=None, op0=ALU.mult),
                     reads=[bst], writes=[bst])
                k.op("dve", lambda e: e.tensor_tensor(out=bst.ap[:, 4:5], in0=bst.ap[:, 1:2], in1=bst.ap[:, 3:4], op=ALU.add),
                     reads=[bst], writes=[bst])
                k.op("dve", lambda e: e.tensor_scalar(out=nmid.ap[:, 0:1], in0=bst.ap[:, 4:5], scalar1=-1.0, scalar2=None, op0=ALU.mult),
                     reads=[bst], writes=[nmid])
                k.op("act", lambda e: e.activation(out=ajunk.ap[:, 0:NW - NH], in_=IfB, func=AF.Sign, bias=nmid.ap[:, 0:1], scale=1.0,
                                                   accum_out=acnt.ap[:, 0:1]),
                     reads=used + [nmid], writes=[ajunk, acnt])
                k.op("dve", lambda e: e.tensor_scalar(out=cjunk.ap[:, 0:NH], in0=IfA, scalar1=bst.ap[:, 4:5], scalar2=0.0,
                                                      op0=ALU.is_ge, op1=ALU.add, accum_out=bst.ap[:, 5:6]),
                     reads=used + [bst], writes=[cjunk, bst])
                k.op("dve", lambda e: e.scalar_tensor_tensor(out=bst.ap[:, 7:8], in0=acnt.ap[:, 0:1], scalar=0.5, in1=bst.ap[:, 5:6],
                                                             op0=ALU.mult, op1=ALU.add), reads=[acnt, bst], writes=[bst])
                k.op("dve", lambda e: e.tensor_scalar(out=bst.ap[:, 6:7], in0=bst.ap[:, 7:8], scalar1=thr_c, scalar2=bst.ap[:, 3:4],
                                                      op0=ALU.is_ge, op1=ALU.mult), reads=[bst], writes=[bst])
                k.op("dve", lambda e: e.tensor_tensor(out=bst.ap[:, 1:2], in0=bst.ap[:, 1:2], in1=bst.ap[:, 6:7], op=ALU.add),
                     reads=[bst], writes=[bst])
            th.append(g)
        for ti, (sb0, nb) in enumerate(tiles):
            def mk(ti=ti, W=nb * 128):
                k.op("dve", lambda e: e.tensor_scalar(out=Mn[ti].ap[:, 0:W], in0=Itile[ti].ap[:, 0:W], scalar1=bst.ap[:, 1:2], scalar2=NEG,
                                                      op0=ALU.is_lt, op1=ALU.mult), reads=[Itile[ti], bst], writes=[Mn[ti]])
            th.append(mk)
        return th

    def att_thunks(i):
        par = i % 2
        NS, tiles = tiles_of(i)
        nt = len(tiles)
        t0 = i * 128
        Mn = Mn1
        qaT, qbT = qaT2[i % 2], qbT2[i % 2]
        qav = qaT.ap.rearrange("p (h t) -> p h t", h=8)
        qbv = qbT.ap.rearrange("p (h t) -> p h t", h=8)
        rs = rs2[i % 2]
        rsv = rs.ap.rearrange("p (h t) -> p h t", h=8)
        th = []

        def prologue():
            k.dma(qaT.ap[0:64, :].rearrange("p (h t) -> p h t", h=8), QA_T[:, :, t0:t0 + 128].rearrange("h d t -> d h t"), writes=[qaT])
            k.dma(qbT.ap[0:64, :].rearrange("p (h t) -> p h t", h=8), QB_T[:, :, t0:t0 + 128].rearrange("h d t -> d h t"), writes=[qbT])
            k.dma(vlast.ap, VB[128 + (NS - 1) * 128:128 + NS * 128, :], writes=[vlast])
            k.op("pe", lambda e: e.matmul(OA_ps.ap, lhsT=onehot_b, rhs=zt512.ap, start=True, stop=False), reads=[cstb, zt512], writes=[OA_ps])
            k.op("pe", lambda e: e.matmul(OB_ps.ap, lhsT=onehot_b, rhs=vlast.ap, start=True, stop=False), reads=[cstb, vlast], writes=[OB_ps])
        th.append(prologue)
        chains = []
        tinfo = {}
        for oi_, ti in enumerate(range(nt - 1, -1, -1)):
            for br in (("dsa", "sb") if (oi_ + i) % 2 == 0 else ("sb", "dsa")):
                for h in range(H):
                    chains.append({"oi": oi_, "ti": ti, "br": br, "h": h})
        carry = [False] * H

        def stage1(c):
            oi_, ti, br, h = c["oi"], c["ti"], c["br"], c["h"]
            sb0, nb = tiles[ti]
            W = nb * 128
            if ti not in tinfo:
                sl = cnt["kv"] % 2
                cnt["kv"] += 1
                ka, kb, va, dv = kaT[sl], kbT[sl], vat[sl], dvt[sl]
                k.dma(ka.ap[0:64, :].rearrange("p (h s) -> p h s", h=8)[:, :, 0:W],
                      KA_T[:, :, sb0 * 128:sb0 * 128 + W].rearrange("h d s -> d h s"), writes=[ka])
                k.dma(va.ap.rearrange("p (j c) -> p j c", j=4)[:, 0:nb, :],
                      VA[sb0 * 128:sb0 * 128 + W, :].rearrange("(j p) c -> p j c", p=128), writes=[va])
                k.dma(kb.ap[0:64, :].rearrange("p (h s) -> p h s", h=8)[:, :, 0:W],
                      KB_T[:, :, sb0 * 128:sb0 * 128 + W].rearrange("h d s -> d h s"), writes=[kb])
                k.dma(dv.ap.rearrange("p (j c) -> p j c", j=4)[:, 0:nb, :],
                      DVB[sb0 * 128:sb0 * 128 + W, :].rearrange("(j p) c -> p j c", p=128), writes=[dv])
                tinfo[ti] = (ka, kb, va, dv)
            ka, kb, va, dv = tinfo[ti]
            kav = ka.ap.rearrange("p (h s) -> p h s", h=8)
            kbv = kb.ap.rearrange("p (h s) -> p h s", h=8)
            first = (oi_ == 0)
            ps = S_ps[cnt["s"] % 4]
            cnt["s"] += 1
            c["nb"] = nb
            if br == "dsa":
                k.op("pe", lambda e: e.matmul(ps.ap[:, 0:W], lhsT=qav[:, h, :], rhs=kav[:, h, 0:W], start=True, stop=False),
                     reads=[qaT, ka], writes=[ps])
                k.op("pe", lambda e: e.matmul(ps.ap[:, 0:W], lhsT=ident_b, rhs=Mn[ti].ap[:, 0:W], start=False, stop=True),
                     reads=[cstb, Mn[ti]], writes=[ps])
                pb = Pb[cnt["pb"] % 6]
                cnt["pb"] += 1
                k.op("act", lambda e: e.activation(out=pb.ap[:, 0:W], in_=ps.ap[:, 0:W], func=AF.Exp, accum_out=rsv[:, h, ti:ti + 1]),
                     reads=[ps], writes=[pb, rs])
                c["src"] = pb
                c["O"] = OA_ps
                c["V"] = va
            else:
                k.op("pe", lambda e: e.matmul(ps.ap[:, 0:W], lhsT=qbv[:, h, :], rhs=kbv[:, h, 0:W], start=True, stop=(not first)),
                     reads=[qbT, kb], writes=[ps])
                if first:
                    k.op("pe", lambda e: e.matmul(ps.ap[:, W - 256:W], lhsT=ident_b, rhs=maskS[par], start=False, stop=True),
                         reads=[cstb], writes=[ps])
                gm = gam[cnt["gm"] % 6]
                hbuf = Hb[cnt["gm"] % 6]
                cnt["gm"] += 1
                k.op("act", lambda e: e.activation(out=gm.ap[:, 0:W], in_=ps.ap[:, 0:W], func=AF.Sigmoid, scale=-1.0),
                     reads=[ps], writes=[gm])
                if not carry[h]:
                    k.op("dve", lambda e: e.tensor_tensor_scan(out=hbuf.ap[:, 0:W][:, ::-1], data0=gm.ap[:, 0:W][:, ::-1],
                                                               data1=gm.ap[:, 0:W][:, ::-1], initial=1.0, op0=ALU.mult, op1=ALU.bypass),
                         reads=[gm], writes=[hbuf])
                else:
                    k.op("dve", lambda e: e.tensor_tensor_scan(out=hbuf.ap[:, 0:W][:, ::-1], data0=gm.ap[:, 0:W][:, ::-1],
                                                               data1=gm.ap[:, 0:W][:, ::-1], initial=cry_l[h].ap[:, 0:1],
                                                               op0=ALU.mult, op1=ALU.bypass),
                         reads=[gm, cry_l[h]], writes=[hbuf])
                if oi_ != nt - 1:
                    k.op("pool", lambda e: e.tensor_copy(out=cry_l[h].ap[:, 0:1], in_=hbuf.ap[:, 0:1]), reads=[hbuf], writes=[cry_l[h]])
                    carry[h] = True
                c["src"] = hbuf
                c["O"] = OB_ps
                c["V"] = dv

        def stage2(c):
            nb, src = c["nb"], c["src"]
            W = nb * 128
            tp = T_ps[cnt["t"] % 2]
            pt = PT[cnt["t"] % 4]
            cnt["t"] += 1
            for j in range(nb):
                k.op("pe", lambda e, j=j: e.matmul(tp.ap[:, j * 128:(j + 1) * 128], lhsT=src.ap[:, j * 128:(j + 1) * 128], rhs=ident_b,
                                                   start=True, stop=True), reads=[src, cstb], writes=[tp])
            if cnt["cp"] % 8 == 0:
                k.op("dve", lambda e: e.tensor_copy(out=pt.ap[:, 0:W], in_=tp.ap[:, 0:W]), reads=[tp], writes=[pt])
            else:
                k.op("act", lambda e: e.activation(out=pt.ap[:, 0:W], in_=tp.ap[:, 0:W], func=AF.Identity), reads=[tp], writes=[pt])
            cnt["cp"] += 1
            c["pt"] = pt

        def stage3(c):
            nb, pt, O_ps, vt, h = c["nb"], c["pt"], c["O"], c["V"], c["h"]
            vv = vt.ap.rearrange("p (j c) -> p j c", j=4)
            for j in range(nb):
                k.op("pe", lambda e, j=j: e.matmul(O_ps.ap[:, h * 64:(h + 1) * 64], lhsT=pt.ap[:, j * 128:(j + 1) * 128],
                                                   rhs=vv[:, j, h * 64:(h + 1) * 64], start=False, stop=False),
                     reads=[pt, vt], writes=[O_ps])

        nch = len(chains)
        for n in range(nch + 4):
            def step(n=n):
                if n < nch:
                    stage1(chains[n])
                if 0 <= n - 2 < nch:
                    stage2(chains[n - 2])
                if 0 <= n - 4 < nch:
                    stage3(chains[n - 4])
            th.append(step)

        def epilogue():
            k.op("dve", lambda e: e.tensor_reduce(out=rst.ap[:, 0:8], in_=rsv[:, :, 0:nt], axis=AX.X, op=ALU.add), reads=[rs], writes=[rst])
            k.op("dve", lambda e: e.reciprocal(out=rst.ap[:, 0:8], in_=rst.ap[:, 0:8]), reads=[rst], writes=[rst])
            for h in range(H):
                k.op("dve", lambda e, h=h: e.tensor_scalar(out=oab.ap[:, h * 64:(h + 1) * 64], in0=OA_ps.ap[:, h * 64:(h + 1) * 64],
                                                         scalar1=rst.ap[:, h:h + 1], scalar2=None, op0=ALU.mult),
                     reads=[OA_ps, rst], writes=[oab])
            k.op("act", lambda e: e.activation(out=oab.ap[:, 512:1024], in_=OB_ps.ap, func=AF.Identity), reads=[OB_ps], writes=[oab])
            ot = oabT[i % 2]
            otv = ot.ap.rearrange("p (c t) -> p c t", c=8)
            for half in range(2):
                tp = T_ps[cnt["t"] % 2]
                cnt["t"] += 1
                for q in range(4):
                    c_ = half * 4 + q
                    k.op("pe", lambda e, tp=tp, q=q, c_=c_: e.matmul(tp.ap[:, q * 128:(q + 1) * 128], lhsT=oab.ap[:, c_ * 128:(c_ + 1) * 128],
                                                                    rhs=ident_b, start=True, stop=True), reads=[oab, cstb], writes=[tp])
                k.op("act", lambda e, tp=tp, half=half: e.activation(out=ot.ap[:, half * 512:(half + 1) * 512], in_=tp.ap, func=AF.Identity),
                     reads=[tp], writes=[ot])
            k.dma(OAB_T[:, :, t0:t0 + 128].rearrange("c p t -> p c t"), otv, reads=[ot])
        th.append(epilogue)
        return th

    def split_idx(i):
        th = idx_thunks(i)
        ntl = len(tiles_of(i)[1])
        return th[:-ntl], th[-ntl:]

    main0, tail0 = split_idx(0)
    for f_ in main0 + tail0:
        f_()
    for i in range(NQB_RUN):
        att = att_thunks(i)
        idx, tail = split_idx(i + 1) if i + 1 < NQB_RUN else ([], [])
        na, ni_ = len(att), len(idx)
        done = 0
        for a_i, f_ in enumerate(att):
            f_()
            want = ((a_i + 1) * ni_) // na
            while done < want:
                idx[done]()
                done += 1
        while done < ni_:
            idx[done]()
            done += 1
        for f_ in tail:
            f_()
    k.barrier()
    A.reset(m)

    m = A.mark()
    stage = [B(1024, F32, "wstage%d" % i) for i in range(2)]
    Wup = B(8 * D, BF16, "Wup")
    Wo = B(8 * D, BF16, "Wo")
    load_weight_bf16(Wup, w_up, 8, D, stage)
    load_weight_bf16(Wo, w_out, 8, D, stage)
    Wupv = Wup.ap.rearrange("p (k n) -> p k n", k=8)
    Wov = Wo.ap.rearrange("p (k n) -> p k n", k=8)
    oT = B(8 * 512, BF16, "oT")
    gaT = B(8 * 512, BF16, "gaT")
    gbT = B(8 * 512, BF16, "gbT")
    mg = B(8 * 512, BF16, "mg")
    oTv = oT.ap.rearrange("p (c t) -> p c t", c=8)
    gav = gaT.ap.rearrange("p (c t) -> p c t", c=8)
    gbv = gbT.ap.rearrange("p (c t) -> p c t", c=8)
    mgv = mg.ap.rearrange("p (c t) -> p c t", c=8)
    u1 = [B(512, F32, "u1_%d" % i) for i in range(2)]
    u2 = [B(512, F32, "u2_%d" % i) for i in range(2)]
    xs = [B(D, F32, "xs%d" % i) for i in range(2)]
    x1s = [B(D, F32, "x1s%d" % i) for i in range(2)]
    junk = B(512, BF16, "junk")
    stat = B(8, F32, "stat")

    def rms_epilogue(psy, xin, xout, Abc, stat):
        for n in range(2):
            k.op("act", lambda e, n=n: e.activation(out=junk.ap, in_=psy[n].ap, func=AF.Square, accum_out=stat.ap[:, n:n + 1]),
                 reads=[psy[n]], writes=[junk, stat])
        k.op("dve", lambda e: e.tensor_tensor(out=stat.ap[:, 2:3], in0=stat.ap[:, 0:1], in1=stat.ap[:, 1:2], op=ALU.add), reads=[stat], writes=[stat])
        k.op("act", lambda e: e.activation(out=stat.ap[:, 3:4], in_=stat.ap[:, 2:3], func=AF.Sqrt, scale=1.0 / D, bias=cols.ap[:, 32:33]),
             reads=[stat, cols], writes=[stat])
        k.op("dve", lambda e: e.reciprocal(out=stat.ap[:, 4:5], in_=stat.ap[:, 3:4]), reads=[stat], writes=[stat])
        for n in range(2):
            k.op("dve", lambda e, n=n: e.scalar_tensor_tensor(out=xout.ap[:, n * 512:(n + 1) * 512], in0=psy[n].ap, scalar=stat.ap[:, 4:5],
                                                             in1=Abc.ap[:, n * 512:(n + 1) * 512], op0=ALU.mult, op1=ALU.mult),
                 reads=[psy[n], stat, Abc], writes=[xout])
        k.op("pool", lambda e: e.tensor_tensor(out=xout.ap, in0=xout.ap, in1=xin.ap, op=ALU.add), reads=[xout, xin], writes=[xout])

    ci = 0
    for ti in range(QT_RUN):
        tok0 = ti * 512
        k.dma(oTv, OAB_T[:, :, tok0:tok0 + 512].rearrange("c p t -> p c t"), writes=[oT])
        k.dma(gav, GA_T[:, :, tok0:tok0 + 512].rearrange("c p t -> p c t"), writes=[gaT])
        k.dma(gbv, GB_T[:, :, tok0:tok0 + 512].rearrange("c p t -> p c t"), writes=[gbT])
        for c in range(8):
            pa, pb_ = PS[(ci % 2) * 2], PS[(ci % 2) * 2 + 1]
            a1, a2 = u1[ci % 2], u2[ci % 2]
            ci += 1
            for kk in range(4):
                k.op("pe", lambda e, pa=pa, kk=kk, c=c: e.matmul(pa.ap, lhsT=Wupv[:, kk, c * 128:(c + 1) * 128], rhs=oTv[:, kk, :],
                                                                start=(kk == 0), stop=(kk == 3)), reads=[Wup, oT], writes=[pa])
            for kk in range(4):
                k.op("pe", lambda e, pb_=pb_, kk=kk, c=c: e.matmul(pb_.ap, lhsT=Wupv[:, 4 + kk, c * 128:(c + 1) * 128], rhs=oTv[:, 4 + kk, :],
                                                                  start=(kk == 0), stop=(kk == 3)), reads=[Wup, oT], writes=[pb_])
            k.op("dve", lambda e, pa=pa, a1=a1, c=c: e.tensor_tensor(out=a1.ap, in0=pa.ap, in1=gav[:, c, :], op=ALU.mult), reads=[pa, gaT], writes=[a1])
            k.op("dve", lambda e, pb_=pb_, a2=a2, c=c: e.tensor_tensor(out=a2.ap, in0=pb_.ap, in1=gbv[:, c, :], op=ALU.mult), reads=[pb_, gbT], writes=[a2])
            k.op("pool", lambda e, a1=a1, a2=a2, c=c: e.tensor_tensor(out=mgv[:, c, :], in0=a1.ap, in1=a2.ap, op=ALU.add), reads=[a1, a2], writes=[mg])
        for sbi in range(4):
            psy = [PS[4 + (sbi % 2) * 2], PS[5 + (sbi % 2) * 2]]
            xin, xo = xs[sbi % 2], x1s[sbi % 2]
            k.dma(xin.ap, xown[tok0 + sbi * 128:tok0 + (sbi + 1) * 128, :], writes=[xin])
            for n in range(2):
                for c in range(8):
                    k.op("pe", lambda e, n=n, c=c, sbi=sbi, psy=psy: e.matmul(psy[n].ap, lhsT=mgv[:, c, sbi * 128:(sbi + 1) * 128],
                                                                             rhs=Wov[:, c, n * 512:(n + 1) * 512], start=(c == 0), stop=(c == 7)),
                         reads=[mg, Wo], writes=[psy[n]])
            rms_epilogue(psy, xin, xo, A1bc, stat)
            k.dma(X1[tok0 + sbi * 128:tok0 + (sbi + 1) * 128, :], xo.ap, reads=[xo])
    k.barrier()
    A.reset(m)

    m = A.mark()
    stage = [B(1024, F32, "wstage%d" % i) for i in range(2)]
    W1 = B(8 * DFF, BF16, "W1")
    W2 = B(32 * D, BF16, "W2")
    load_weight_bf16(W1, w_ff1, 8, DFF, stage)
    load_weight_bf16(W2, w_ff2, 32, D, stage)
    W1v = W1.ap.rearrange("p (k n) -> p k n", k=8)
    W2v = W2.ap.rearrange("p (k n) -> p k n", k=32)
    TT = 256
    x1t = [B(D, F32, "x1t%d" % i) for i in range(2)]
    junk2 = B(D, BF16, "junk2")
    junk = B(512, BF16, "junk")
    stat2 = B(4, F32, "stat2")
    stat = B(8, F32, "stat")
    hn = B(D, BF16, "hn")
    h2T = B(8 * TT, BF16, "h2T")
    h2v = h2T.ap.rearrange("p (k n) -> p k n", k=8)
    uT = B(32 * TT, BF16, "uT")
    uTv = uT.ap.rearrange("p (f t) -> p f t", f=32)
    rl = [B(TT, BF16, "rl%d" % i) for i in range(2)]
    outs = [B(D, F32, "outs%d" % i) for i in range(2)]
    for ti in range(QT_RUN * 2):
        tok0 = ti * TT
        for sbi in range(2):
            x_ = x1t[sbi]
            k.dma(x_.ap, X1[tok0 + sbi * 128:tok0 + (sbi + 1) * 128, :], writes=[x_])
            norm_transpose(x_, h2T, 0, sbi, TT, junk2, stat2, hn, 2)
        for f in range(32):
            ps = PS[f % 2]
            for kk in range(8):
                k.op("pe", lambda e, ps=ps, kk=kk, f=f: e.matmul(ps.ap[:, 0:TT], lhsT=W1v[:, kk, f * 128:(f + 1) * 128], rhs=h2v[:, kk, :],
                                                                start=(kk == 0), stop=(kk == 7)), reads=[W1, h2T], writes=[ps])
            r_ = rl[f % 2]
            k.op("act", lambda e, ps=ps, r_=r_: e.activation(out=r_.ap, in_=ps.ap[:, 0:TT], func=AF.Relu), reads=[ps], writes=[r_])
            k.op("dve", lambda e, r_=r_, f=f: e.tensor_tensor(out=uTv[:, f, :], in0=r_.ap, in1=r_.ap, op=ALU.mult), reads=[r_], writes=[uT])
        for sbi in range(2):
            psy = [PS[2 + sbi * 2], PS[3 + sbi * 2]]
            for n in range(2):
                for f in range(32):
                    k.op("pe", lambda e, n=n, f=f, sbi=sbi, psy=psy: e.matmul(psy[n].ap, lhsT=uTv[:, f, sbi * 128:(sbi + 1) * 128],
                                                                             rhs=W2v[:, f, n * 512:(n + 1) * 512], start=(f == 0), stop=(f == 31)),
                         reads=[uT, W2], writes=[psy[n]])
            o_ = outs[sbi]
            rms_epilogue(psy, x1t[sbi], o_, A2bc, stat)
            k.dma(out_own[tok0 + sbi * 128:tok0 + (sbi + 1) * 128, :], o_.ap, reads=[o_])
    k.barrier()
    A.reset(m)
    k.replay()
    return nc


def _off(ap):
    return ap.offset


def _own_blocks(r):
    return [2 * i + ((i % 2) if r == 0 else (1 - i % 2)) for i in range(NQB)]


def _rope_tables(pos):
    inv = (500000.0 ** (-(np.arange(0, 16, 2, dtype=np.float32) / 16.0))).astype(np.float32)
    ang = pos.astype(np.float32)[:, None] * inv[None, :]
    c = np.cos(ang).astype(np.float32)
    s = np.sin(ang).astype(np.float32)
    T = pos.shape[0]
    cf = np.ones((64, T), np.float32)
    sf = np.zeros((64, T), np.float32)
    cf[0:8] = c.T
    cf[8:16] = c.T
    sf[0:8] = -s.T
    sf[8:16] = s.T
    return cf, sf


_PERM = np.concatenate([np.arange(8, 16), np.arange(0, 8), np.arange(16, 64)])


def _consts(r):
    cst = np.zeros((128, 1536), np.float32)
    cst[:, 0:128] = np.eye(128, dtype=np.float32)
    cst[127, 128:256] = 1.0
    tl = np.arange(128)[:, None]
    sl = np.arange(128)[None, :]
    BIG = 1e30
    diag_incl = np.where(sl <= tl, 0.0, 1.0).astype(np.float32)
    diag_strict = np.where(sl < tl, 0.0, 1.0).astype(np.float32)
    full = np.ones((128, 128), np.float32)
    none = np.zeros((128, 128), np.float32)
    for par in range(2):
        off = par if r == 0 else 1 - par
        if off == 0:
            mi = np.concatenate([diag_incl, full], axis=1)
            ms = np.concatenate([diag_strict, full], axis=1)
        else:
            mi = np.concatenate([none, diag_incl], axis=1)
            ms = np.concatenate([none, diag_strict], axis=1)
        cst[:, 256 + par * 256:512 + par * 256] = mi * (-BIG)
        cst[:, 768 + par * 256:1024 + par * 256] = ms * NEG
    cst[:, 1280:1408] = 1.0
    return cst


def kernel(x, c, w_ada, b_ada, g_pre_mix, w_in, w_up_a, w_up_b, w_out, g_post_mix, g_pre_ffn, w_ff1, w_ff2, g_post_ffn):
    debug = bool(os.environ.get("MK_DEBUG"))
    f = np.float32
    x = np.asarray(x, f)
    w_in0 = np.asarray(w_in, f)[0]
    o = np.cumsum([0, 512, 512, 512, 512, 512, 512, 512, 64, 8, 1024, 1024])
    qa_c, ka_c, va_c, qb_c, kb_c, vb_c, qi_c, ki_c, wi_c, ga_c, gb_c = [np.arange(o[j], o[j + 1]) for j in range(11)]

    def rope_pairs(cols, nh):
        out = []
        for p in range(nh // 2):
            h0 = cols[(2 * p) * 64:(2 * p + 1) * 64]
            h1 = cols[(2 * p + 1) * 64:(2 * p + 2) * 64]
            out += [h0, h1, h0[_PERM], h1[_PERM]]
        return np.concatenate(out)

    ki_ext = np.concatenate([ki_c, ki_c, ki_c[_PERM], ki_c[_PERM]])
    colsK = np.concatenate([rope_pairs(ka_c, 8), kb_c, ki_ext, va_c, vb_c])
    colsQ = np.concatenate([rope_pairs(qa_c, 8), qb_c, rope_pairs(qi_c, 8), ga_c, gb_c, wi_c])
    assert colsK.shape[0] == NK and colsQ.shape[0] == NQ
    WK = np.ascontiguousarray(w_in0[:, colsK])
    WQ = np.ascontiguousarray(w_in0[:, colsQ])
    w_up = np.ascontiguousarray(np.concatenate([np.asarray(w_up_a, f)[0], np.asarray(w_up_b, f)[0]], axis=0))
    g_rows = np.ascontiguousarray(np.stack([np.asarray(g_pre_mix, f)[0], np.asarray(g_post_mix, f)[0],
                                            np.asarray(g_pre_ffn, f)[0], np.asarray(g_post_ffn, f)[0]], axis=0))
    cosK, sinK = _rope_tables(np.arange(L))
    in_maps = []
    own_rows = []
    for core in range(8):
        b, r = core // 2, core % 2
        blocks = _own_blocks(r)
        rows = np.concatenate([np.arange(g * 128, (g + 1) * 128) for g in blocks])
        own_rows.append(rows)
        cq, sq = _rope_tables(rows)
        in_maps.append({
            "xall": np.ascontiguousarray(x[b]),
            "xown": np.ascontiguousarray(x[b][rows]),
            "ccol": np.ascontiguousarray(np.asarray(c, f)[b].reshape(8, 128).T),
            "w_ada": np.ascontiguousarray(np.asarray(w_ada, f)[0]),
            "b_ada": np.ascontiguousarray(np.asarray(b_ada, f)[0:1]),
            "g_rows": g_rows,
            "WK": WK, "WQ": WQ, "w_up": w_up,
            "w_out": np.ascontiguousarray(np.asarray(w_out, f)[0]),
            "w_ff1": np.ascontiguousarray(np.asarray(w_ff1, f)[0]),
            "w_ff2": np.ascontiguousarray(np.asarray(w_ff2, f)[0]),
            "cosK": cosK, "sinK": sinK,
            "cosQ": np.ascontiguousarray(cq * 0.125), "sinQ": np.ascontiguousarray(sq * 0.125),
            "cosI": cq, "sinI": sq,
            "consts": _consts(r),
        })
    nc = build_program(debug)
    res = run_bass_kernel_spmd(nc, in_maps, core_ids=list(range(8)))
    out = np.zeros((4, L, D), np.float32)
    for core in range(8):
        out[core // 2][own_rows[core]] = res.results[core]["out_own"]
    if debug:
        kernel._dbg = res.results
    return out
```
